# Optimizing a Trainium2 kernel written in Bass

```python
import jax, jax.numpy as jnp
from jax import lax
import numpy as np

D_MODEL = 1024
BATCH = 4
SEQ = 8192
DEPTH = 2

D_MIX = D_MODEL
D_CONV = D_MIX // 2
CONV_HEADS = 8
CONV_WIDTH = 3
D_POOL = D_MIX - D_CONV
POOL_WINDOWS = (2, 4, 8, 16)
N_POOL_GROUPS = len(POOL_WINDOWS)
D_POOL_GROUP = D_POOL // N_POOL_GROUPS
D_IN_PROJ = 3 * D_CONV + D_POOL
N_EXPERT_GROUPS = 4
EXPERTS_PER_GROUP = 8
N_EXPERTS = N_EXPERT_GROUPS * EXPERTS_PER_GROUP
TOP_K = 2
D_EXPERT = D_MODEL // 2
EXPERT_BLOCK = 128
D_PLE = 256
DEEPNORM_ALPHA = (2 * DEPTH) ** 0.25
DEEPNORM_BETA = (8 * DEPTH) ** -0.25
LN_EPS = 1e-5

kernel_name = "hybrid_conv_pool_hmoe_encoder"


def _layernorm(x, g, b):
    xf = x.astype(jnp.float32)
    mu = jnp.mean(xf, axis=-1, keepdims=True)
    var = jnp.mean(jnp.square(xf - mu), axis=-1, keepdims=True)
    y = (xf - mu) * lax.rsqrt(var + LN_EPS)
    return (y * g.astype(jnp.float32) + b.astype(jnp.float32)).astype(x.dtype)


def _centred_mean_minus_self(u, window):
    S = u.shape[1]
    left = window // 2
    right = window - 1 - left
    cs = jnp.cumsum(u, axis=1)
    cs0 = jnp.pad(cs, ((0, 0), (1, 0), (0, 0)))
    cs_ext = jnp.pad(cs0, ((0, 0), (left, right), (0, 0)), mode="edge")
    hi = cs_ext[:, window:window + S]
    lo = cs_ext[:, :S]
    t = jnp.arange(S)
    cnt = (jnp.minimum(t + right + 1, S) - jnp.maximum(t - left, 0)).astype(jnp.float32)
    return (hi - lo) / cnt[None, :, None] - u


def _hybrid_mixer(xn, w_in, b_in, conv_w, pool_w, pool_scale, w_o):
    B, S, _ = xn.shape
    z = xn @ w_in + b_in
    h = z[..., :D_CONV]
    gate_b = z[..., D_CONV:2 * D_CONV]
    gate_c = z[..., 2 * D_CONV:3 * D_CONV]
    u = z[..., 3 * D_CONV:]
    v = gate_c * h
    v = lax.conv_general_dilated(
        v, conv_w[:, None, :], window_strides=(1,), padding=((1, 1),),
        dimension_numbers=("NWC", "WIO", "NWC"), feature_group_count=D_CONV)
    y_conv = gate_b * v
    ug = u.astype(jnp.float32).reshape(B, S, N_POOL_GROUPS, D_POOL_GROUP)
    pooled = jnp.stack(
        [_centred_mean_minus_self(ug[:, :, g], w) for g, w in enumerate(POOL_WINDOWS)],
        axis=2).astype(xn.dtype)
    y_pool = jnp.einsum("bsgc,gcd->bsgd", pooled, pool_w).reshape(B, S, D_POOL) * pool_scale
    return jnp.concatenate([y_conv, y_pool], axis=-1) @ w_o


def _hierarchical_moe(xn, w_rg, b_rg, w_re, b_re, w1, w3, w2):
    B, S, D = xn.shape
    N = B * S
    xf = xn.reshape(N, D)
    x32 = xf.astype(jnp.float32)
    g_probs = jax.nn.softmax(x32 @ w_rg.astype(jnp.float32) + b_rg.astype(jnp.float32), axis=-1)
    g_sel = jnp.argmax(g_probs, axis=-1)
    g_w = jnp.take_along_axis(g_probs, g_sel[:, None], axis=1)[:, 0]
    e_logits = jnp.einsum("nd,gde->nge", x32, w_re.astype(jnp.float32)) + b_re.astype(jnp.float32)
    e_logits = jnp.take_along_axis(e_logits, g_sel[:, None, None], axis=1)[:, 0]
    top_val, top_idx = lax.top_k(e_logits, TOP_K)
    top_w = jax.nn.softmax(top_val, axis=-1) * g_w[:, None]
    expert_id = (g_sel[:, None] * EXPERTS_PER_GROUP + top_idx).reshape(-1).astype(jnp.int32)
    token_id = jnp.repeat(jnp.arange(N, dtype=jnp.int32), TOP_K)
    gate_w = top_w.reshape(-1)
    A = N * TOP_K
    n_slots = -(-(A + N_EXPERTS * (EXPERT_BLOCK - 1)) // EXPERT_BLOCK) * EXPERT_BLOCK
    n_blocks = n_slots // EXPERT_BLOCK
    order = jnp.argsort(expert_id)
    e_sorted = expert_id[order]
    counts = jnp.bincount(expert_id, length=N_EXPERTS).astype(jnp.int32)
    start = jnp.cumsum(counts) - counts
    padded = (counts + EXPERT_BLOCK - 1) // EXPERT_BLOCK * EXPERT_BLOCK
    padded_end = jnp.cumsum(padded)
    padded_start = padded_end - padded
    rank = jnp.arange(A, dtype=jnp.int32) - start[e_sorted]
    dest = padded_start[e_sorted] + rank
    slot_token = jnp.full((n_slots,), N, jnp.int32).at[dest].set(token_id[order])
    slot_w = jnp.zeros((n_slots,), jnp.float32).at[dest].set(gate_w[order])
    block_starts = jnp.arange(n_blocks, dtype=jnp.int32) * EXPERT_BLOCK
    block_expert = jnp.minimum(jnp.searchsorted(padded_end, block_starts, side="right"),
                               N_EXPERTS - 1).astype(jnp.int32)
    x_pad = jnp.concatenate([xf, jnp.zeros((1, D), xf.dtype)], axis=0)
    xs = x_pad[slot_token].reshape(n_blocks, EXPERT_BLOCK, D)

    def expert_block(args):
        xb, e = args
        hb = jax.nn.silu(xb @ w1[e]) * (xb @ w3[e])
        return hb @ w2[e]

    ys = lax.map(expert_block, (xs, block_expert)).reshape(n_slots, D)
    ys = ys * slot_w.astype(ys.dtype)[:, None]
    out = jax.ops.segment_sum(ys, slot_token, num_segments=N + 1)[:N]
    return out.reshape(B, S, D)


def setup_inputs(seed: int = 0) -> dict:
    key = jax.random.key(seed)
    ks = jax.random.split(key, 32)
    L, D = DEPTH, D_MODEL

    def nrm(k, shape, scale):
        return jax.random.normal(k, shape, jnp.float32) * scale

    return {
        "x": nrm(ks[0], (BATCH, SEQ, D), 1.0),
        "p": nrm(ks[1], (DEPTH, BATCH, SEQ, D_PLE), 1.0),
        "ln0_g": 1.0 + nrm(ks[2], (D,), 0.02),
        "ln0_b": nrm(ks[3], (D,), 0.02),
        "w_in": nrm(ks[4], (L, D, D_IN_PROJ), D ** -0.5),
        "b_in": nrm(ks[5], (L, D_IN_PROJ), 0.02),
        "conv_w": nrm(ks[6], (L, CONV_WIDTH, D_CONV), CONV_WIDTH ** -0.5),
        "pool_w": nrm(ks[7], (L, N_POOL_GROUPS, D_POOL_GROUP, D_POOL_GROUP), D_POOL_GROUP ** -0.5),
        "pool_scale": 1.0 + nrm(ks[8], (L, D_POOL), 0.02),
        "w_o": nrm(ks[9], (L, D_MIX, D), DEEPNORM_BETA * D_MIX ** -0.5),
        "ln1_g": 1.0 + nrm(ks[10], (L, D), 0.02),
        "ln1_b": nrm(ks[11], (L, D), 0.02),
        "w_router_group": nrm(ks[12], (L, D, N_EXPERT_GROUPS), D ** -0.5),
        "b_router_group": nrm(ks[13], (L, N_EXPERT_GROUPS), 0.01),
        "w_router_expert": nrm(ks[14], (L, N_EXPERT_GROUPS, D, EXPERTS_PER_GROUP), D ** -0.5),
        "b_router_expert": nrm(ks[15], (L, N_EXPERT_GROUPS, EXPERTS_PER_GROUP), 0.01),
        "w1": nrm(ks[16], (L, N_EXPERTS, D, D_EXPERT), D ** -0.5),
        "w3": nrm(ks[17], (L, N_EXPERTS, D, D_EXPERT), D ** -0.5),
        "w2": nrm(ks[18], (L, N_EXPERTS, D_EXPERT, D), DEEPNORM_BETA * D_EXPERT ** -0.5),
        "w_ple_gate": nrm(ks[19], (L, D, D), D ** -0.5),
        "b_ple_gate": nrm(ks[20], (L, D), 0.02),
        "w_ple_proj": nrm(ks[21], (L, D_PLE, D), DEEPNORM_BETA * D_PLE ** -0.5),
        "ln2_g": 1.0 + nrm(ks[22], (L, D), 0.02),
        "ln2_b": nrm(ks[23], (L, D), 0.02),
    }


def reference(x, p, ln0_g, ln0_b, w_in, b_in, conv_w, pool_w, pool_scale, w_o,
              ln1_g, ln1_b, w_router_group, b_router_group, w_router_expert,
              b_router_expert, w1, w3, w2, w_ple_gate, b_ple_gate, w_ple_proj,
              ln2_g, ln2_b):
    x = _layernorm(x, ln0_g, ln0_b)
    for i in range(DEPTH):
        mix = _hybrid_mixer(x, w_in[i], b_in[i], conv_w[i], pool_w[i], pool_scale[i], w_o[i])
        x = _layernorm(DEEPNORM_ALPHA * x + mix, ln1_g[i], ln1_b[i])
        ffn = _hierarchical_moe(x, w_router_group[i], b_router_group[i], w_router_expert[i],
                                b_router_expert[i], w1[i], w3[i], w2[i])
        ple = jax.nn.sigmoid(x @ w_ple_gate[i] + b_ple_gate[i]) * (p[i] @ w_ple_proj[i])
        x = _layernorm(DEEPNORM_ALPHA * x + ffn + ple, ln2_g[i], ln2_b[i])
    return x
```

```python
from contextlib import ExitStack

import numpy as np
import concourse.bass as bass
import concourse.mybir as mybir
from concourse.bass_utils import run_bass_kernel_spmd

F32 = mybir.dt.float32
BF16 = mybir.dt.bfloat16
I32 = mybir.dt.int32
U32 = mybir.dt.uint32
AF = mybir.ActivationFunctionType
ALU = mybir.AluOpType
AX = mybir.AxisListType

NCORES = 8
D = 1024
DIN = 2048
NE = 32
DE = 512
DPLE = 256
SEQ = 8192
OWN = 4096
CAP = 384
CH = 256
W = CH + 16
DEPTH = 2
ALPHA = float((2 * DEPTH) ** 0.25)
EPS = 1e-5
NT = (34, 32)
NCHUNK = (17, 16)
XROWS = 4368
POOL_W = (2, 4, 8, 16)
BWIN = ((0, 0, 120), (0, 16, 120), (1, 0, 0), (1, 15, 240))
import os
DEBUG = bool(int(os.environ.get("KDEBUG", "0")))
STAGE = os.environ.get("KSTAGE", "all")
KNCH = int(os.environ.get("KNCH", "0"))
KMAXOPS = int(os.environ.get("KMAXOPS", "0"))
MARKS = {}
NE_DECL = 1 if STAGE in ("w", "a0", "a0s") else 32

ENG_ATTR = {"pe": "tensor", "act": "scalar", "dve": "vector", "pool": "gpsimd", "sp": "sync"}
NDMA_SEM = {"sp": 16, "act": 4, "pool": 16}


class Op:
    __slots__ = ("eng", "fn", "deps", "dma", "idx", "sig", "sem", "val", "extra")

    def __init__(self, eng, fn, deps, dma, idx):
        self.eng, self.fn, self.deps, self.dma, self.idx = eng, fn, deps, dma, idx
        self.sig = False
        self.sem = None
        self.val = 0
        self.extra = None


class Sched:
    def __init__(self):
        self.ops = []
        self.lastw = {}
        self.rd_eng = {}
        self.rd_dma = {}
        self.dma_since = []
        self.bar_set = set()
        self.bar_pending = set()

    def barrier(self):
        last = {}
        for op in self.ops:
            last[op.eng] = op.idx
        self.bar_set = set(last.values()) | set(self.dma_since)
        self.dma_since = []
        self.bar_pending = set(ENG_ATTR)

    def add(self, eng, fn, reads=(), writes=(), dma=False):
        i = len(self.ops)
        if KMAXOPS and i >= KMAXOPS:
            return i - 1
        deps = set()
        if eng in self.bar_pending:
            deps |= self.bar_set
            self.bar_pending.discard(eng)
        if dma:
            self.dma_since.append(i)
        for r in reads:
            w = self.lastw.get(r)
            if w is not None:
                deps.add(w)
        for r in writes:
            w = self.lastw.get(r)
            if w is not None:
                deps.add(w)
            for rd in self.rd_eng.get(r, {}).values():
                deps.add(rd)
            for rd in self.rd_dma.get(r, ()):
                deps.add(rd)
        op = Op(eng, fn, deps, dma, i)
        self.ops.append(op)
        for r in reads:
            if dma:
                self.rd_dma.setdefault(r, []).append(i)
            else:
                self.rd_eng.setdefault(r, {})[eng] = i
        for r in writes:
            self.lastw[r] = i
            self.rd_eng[r] = {}
            self.rd_dma[r] = []
        return i

    def emit(self, nc, final_wait_ops=()):
        ops = self.ops
        for op in ops:
            if op.eng == "pe" and not op.dma:
                op.deps = {d for d in op.deps if not (ops[d].eng == "pe" and not ops[d].dma)}
        last_per_eng = {}
        for op in ops:
            last_per_eng[op.eng] = op.idx
        fence = Op("sp", None, set(final_wait_ops) | set(last_per_eng.values()), False, len(ops))
        ops = ops + [fence]
        for op in ops:
            for d in op.deps:
                ops[d].sig = True
        with ExitStack() as es:
            esem = {e: es.enter_context(nc.semaphore("prog_" + e)) for e in ENG_ATTR}
            dsem = {q: [es.enter_context(nc.semaphore(f"dma_{q}_{k}")) for k in range(n)]
                    for q, n in NDMA_SEM.items()}
            cnt = {e: 0 for e in ENG_ATTR}
            ndma = {q: 0 for q in NDMA_SEM}
            slot_prev = {q: [None] * n for q, n in NDMA_SEM.items()}
            for op in ops:
                if op.dma:
                    q = op.eng
                    k = ndma[q]
                    ndma[q] += 1
                    s = k % NDMA_SEM[q]
                    op.sem = dsem[q][s]
                    op.val = 16 * (k // NDMA_SEM[q] + 1)
                    op.extra = slot_prev[q][s]
                    slot_prev[q][s] = op.idx
                elif op.sig:
                    cnt[op.eng] += 1
                    op.sem = esem[op.eng]
                    op.val = cnt[op.eng]
            per_eng = {e: [op for op in ops if op.eng == e] for e in ENG_ATTR}
            block = es.enter_context(nc.Block())

            def make(e):
                def body(eng):
                    known = {}
                    for op in per_eng[e]:
                        need = {}
                        deps = set(op.deps)
                        if op.extra is not None:
                            deps.add(op.extra)
                        for d in deps:
                            dop = ops[d]
                            key = id(dop.sem)
                            if key not in need or need[key][1] < dop.val:
                                need[key] = (dop.sem, dop.val)
                        for key, (sem, val) in need.items():
                            if known.get(key, 0) >= val:
                                continue
                            eng.wait_ge(sem, val)
                            known[key] = val
                        if op.fn is None:
                            continue
                        inst = op.fn(eng)
                        if op.dma:
                            inst.then_inc(op.sem, 16)
                        elif op.sig:
                            inst.then_inc(op.sem, 1)
                return body

            for e, attr in ENG_ATTR.items():
                getattr(block, attr)(make(e))


def build_nc():
    nc = bass.Bass("TRN2", target_bir_lowering=False)
    S = Sched()

    def din(name, shape, dt=F32):
        return nc.dram_tensor(name, list(shape), dt, kind="ExternalInput").ap()

    def dscr(name, shape, dt=F32):
        kind = "ExternalOutput" if (DEBUG and name in ("r2p0", "dbg_route", "xcur")) else "Internal"
        return nc.dram_tensor(name, list(shape), dt, kind=kind).ap()

    xpad = din("xpad", [XROWS, D])
    ppad = din("ppad", [DEPTH, NT[0] * 128, DPLE])
    bmw_d = din("bmw", [1, 640])
    tokinfo_d = din("tokinfo", [128, DEPTH * NT[0] * 2])
    ln0_d = din("ln0", [2, D])
    lnv_d = din("lnv", [DEPTH, 4, D])
    cols_d = din("cols", [DEPTH, 128, 32])
    w_in_d = din("w_in", [DEPTH, D, DIN])
    pool_w_d = din("pool_w", [DEPTH, 4, 128, 128])
    w_o_d = din("w_o", [DEPTH, D, D])
    wr_d = din("wr", [DEPTH, D, 64])
    br_d = din("br", [DEPTH, 1, 64])
    w1_d = din("w1", [DEPTH, NE_DECL, D, DE])
    w3_d = din("w3", [DEPTH, NE_DECL, D, DE])
    w2_d = din("w2", [DEPTH, NE_DECL, DE, D])
    wg_d = din("w_ple_gate", [DEPTH, D, D])
    bg_d = din("b_ple_gate", [DEPTH, 1, D])
    wp_d = din("w_ple_proj", [DEPTH, DPLE, D])
    out_d = nc.dram_tensor("out", [OWN, D], F32, kind="ExternalOutput").ap()

    xcur = dscr("xcur", [XROWS, D])
    r2p_d = [dscr(f"r2p{l}", [NT[0] * 128, D]) for l in range(DEPTH)]
    xs_d = [dscr(f"xs{l}", [NE * CAP + 128, D], BF16) for l in range(DEPTH)]
    ys_d = [dscr(f"ys{l}", [NE * CAP + 128, D]) for l in range(DEPTH)]
    if DEBUG:
        dbg_route = dscr("dbg_route", [DEPTH, 128, NT[0] * 4])

    es = ExitStack()
    with es:
        def sb(name, shape, dt):
            return es.enter_context(nc.sbuf_tensor("s_" + name, list(shape), dt))

        w_in_sb = sb("w_in_sb", [128, 8, DIN], BF16)
        w_o_sb = sb("w_o_sb", [128, 8, D], BF16)
        wg_sb = sb("wg_sb", [128, 8, D], BF16)
        wp_sb = sb("wp_sb", [128, 2, D], BF16)
        poolw_sb = sb("poolw_sb", [128, 4, 128], BF16)
        wr_sb = sb("wr_sb", [128, 8, 64], F32)
        wr_hi = sb("wr_hi", [128, 8, 64], BF16)
        wr_lo = sb("wr_lo", [128, 8, 64], BF16)
        tokinfo = sb("tokinfo", [128, DEPTH * NT[0] * 2], F32)
        br_sb = sb("br_sb", [128, 64], F32)
        bg_sb = sb("bg_sb", [128, D], BF16)
        cols = sb("cols", [128, 32], F32)
        lnA_g = sb("lnA_g", [128, D], F32)
        lnA_b = sb("lnA_b", [128, D], F32)
        lnB_g = sb("lnB_g", [128, D], F32)
        lnB_b = sb("lnB_b", [128, D], F32)
        ident_f = sb("ident_f", [128, 128], F32)
        ident_b = sb("ident_b", [128, 128], BF16)
        lstrict = sb("lstrict", [128, 128], BF16)
        ones_b = sb("ones_b", [128, 128], BF16)
        ones_f = sb("ones_f", [1, 128], F32)
        tmp_f = sb("tmp_f", [128, 128], F32)
        iota32 = sb("iota32", [128, 32], F32)
        iotacap = sb("iotacap", [128, 32], F32)
        iotadiv8 = sb("iotadiv8", [128, 32], F32)
        lg8 = sb("lg8", [128, 8], F32)
        mhalf = sb("mhalf", [128, 1], F32)
        trash = sb("trash", [128, 1], F32)
        carry = sb("carry", [128, 32], F32)
        dest_i = sb("dest_i", [128, NT[0], 2], I32)
        wts = sb("wts", [128, NT[0], 2], F32)
        bmw = sb("bmw_sb", [128, 4, 160], F32)
        small = sb("small", [128, 8, 20], F32)
        rt = sb("rt", [128, 2, 256], F32)
        rtu = sb("rtu", [128, 2, 16], U32)
        mbf = sb("mbf", [128, 2, 64], BF16)

        RBYTES = 104 * 1024
        R = sb("R", [128, RBYTES // 4], F32)
        carve_off = [0]

        def carve(shape, dt, reset=False):
            if reset:
                carve_off[0] = 0
            esz = 4 if dt in (F32, I32, U32) else 2
            n = int(np.prod(shape[1:]))
            nbytes = (n * esz + 31) // 32 * 32
            a = carve_off[0] // 4
            b = (carve_off[0] + nbytes) // 4
            assert carve_off[0] + nbytes <= RBYTES, ("region overflow", carve_off[0] + nbytes)
            carve_off[0] += nbytes
            v = R[:, a:b]
            if esz == 2:
                v = v.bitcast(dt)
            elif dt != F32:
                v = v.bitcast(dt)
            v = v[:, 0:n]
            if len(shape) == 3:
                v = v.rearrange("p (k n) -> p k n", k=shape[1])
            return v

        xn_t = [carve([128, D], F32, reset=(i == 0)) for i in range(4)]
        hx = carve([128, D], F32)
        xnTh = carve([128, 8, 128], BF16)
        xnT = [carve([128, 8, W], BF16) for _ in range(2)]
        zb = [carve([128, W], F32) for _ in range(6)]
        mt = [carve([128, W], F32) for _ in range(6)]
        catT2 = [carve([128, 8, CH], BF16) for _ in range(2)]
        pooledT = [carve([128, CH], BF16) for _ in range(2)]
        r_t = [carve([128, D], F32) for _ in range(1)]
        x1_t = [carve([128, D], F32) for _ in range(2)]
        x1bf = [carve([128, D], BF16) for _ in range(2)]
        x1T32 = carve([128, 8, 128], F32)
        x1Tb = [carve([128, 8, 128], BF16) for _ in range(2)]
        x1Tlo = carve([128, 8, 128], BF16)
        gate_t = [carve([128, D], F32) for _ in range(2)]
        p_t = [carve([128, DPLE], F32) for _ in range(4)]
        xbf_t = [carve([128, D], BF16) for _ in range(2)]
        pT = [carve([128, 2, 128], BF16) for _ in range(2)]
        a_end = carve_off[0]
        w1_sb = [carve([128, 8, DE], BF16, reset=(i == 0)) for i in range(2)]
        w3_sb = [carve([128, 8, DE], BF16) for _ in range(2)]
        w2_sb = [carve([128, 4, D], BF16) for _ in range(2)]
        xs_sb = [carve([128, 3, D], BF16) for _ in range(2)]
        xsT = carve([128, 8, CAP], BF16)
        hT = carve([128, 4, CAP], BF16)
        silu_t = [carve([128, CAP], F32) for _ in range(2)]
        ys_sb = carve([128, 3, D], F32)
        NCB = 4
        cy0 = [carve([128, D], F32, reset=(i == 0)) for i in range(NCB)]
        cy1 = [carve([128, D], F32) for _ in range(NCB)]
        cr = [carve([128, D], F32) for _ in range(NCB)]

        P01 = es.enter_context(nc.psum_tensor("P01", [128, 1024], F32))
        P23 = es.enter_context(nc.psum_tensor("P23", [128, 1024], F32))
        P45 = es.enter_context(nc.psum_tensor("P45", [128, 1024], F32))
        P67 = es.enter_context(nc.psum_tensor("P67", [128, 1024], F32))
        PT_bf = P01[:, 0:512].bitcast(BF16)

        REG = "REGION"

        def dma(q, out, in_, reads=(), writes=()):
            return S.add(q, lambda e: e.dma_start(out=out, in_=in_), reads, writes, dma=True)

        def tt(eng, out, a, b, op, reads, writes):
            return S.add(eng, lambda e: e.tensor_tensor(out=out, in0=a, in1=b, op=op), reads, writes)

        def ts(eng, out, a, s1, s2, op0, op1, reads, writes):
            if s2 is None:
                return S.add(eng, lambda e: e.tensor_scalar(out=out, in0=a, scalar1=s1, scalar2=None, op0=op0), reads, writes)
            return S.add(eng, lambda e: e.tensor_scalar(out=out, in0=a, scalar1=s1, scalar2=s2, op0=op0, op1=op1), reads, writes)

        def stt(eng, out, a, s, b, op0, op1, reads, writes):
            return S.add(eng, lambda e: e.scalar_tensor_tensor(out=out, in0=a, scalar=s, in1=b, op0=op0, op1=op1), reads, writes)

        def actf(out, in_, func, reads, writes, bias=None, scale=None, accum=None):
            kw = {}
            if bias is not None:
                kw["bias"] = bias
            if scale is not None:
                kw["scale"] = scale
            if accum is not None:
                kw["accum_out"] = accum
            return S.add("act", lambda e: e.activation(out=out, in_=in_, func=func, **kw), reads, writes)

        def cp(eng, out, in_, reads, writes):
            if eng == "act":
                return S.add(eng, lambda e: e.activation(out=out, in_=in_, func=AF.Copy), reads, writes)
            return S.add(eng, lambda e: e.tensor_copy(out=out, in_=in_), reads, writes)

        def mm(out, lhsT, rhs, start, stop, reads, writes):
            return S.add("pe", lambda e: e.matmul(out, lhsT=lhsT, rhs=rhs, start=start, stop=stop), reads, writes)

        def tr(out, in_, ident, reads, writes):
            return S.add("pe", lambda e: e.transpose(out=out, in_=in_, identity=ident), reads, writes)

        def scatter(dst, idx, src, reads, writes):
            return S.add("pool", lambda e: e.indirect_dma_start(
                out=dst, out_offset=bass.IndirectOffsetOnAxis(ap=idx, axis=0), in_=src, in_offset=None),
                reads, writes, dma=True)

        def gather(dst, src, idx, reads, writes):
            return S.add("pool", lambda e: e.indirect_dma_start(
                out=dst, out_offset=None, in_=src, in_offset=bass.IndirectOffsetOnAxis(ap=idx, axis=0)),
                reads, writes, dma=True)

        small_ctr = [0]

        def layernorm(src, src_res, dst, dst_res, g_t, b_t, gb_res, beta_eng="pool"):
            k = small_ctr[0] % 8
            small_ctr[0] += 1
            sm = small[:, k, :]
            sres = ("small", k)
            st = sm[:, 0:12]
            mv = sm[:, 12:14]
            std = sm[:, 14:15]
            rstd = sm[:, 15:16]
            nmr = sm[:, 16:17]
            S.add("dve", lambda e: e.bn_stats(out=st[:, 0:6], in_=src[:, 0:512]), [src_res], [sres])
            S.add("dve", lambda e: e.bn_stats(out=st[:, 6:12], in_=src[:, 512:1024]), [src_res, sres], [sres])
            S.add("dve", lambda e: e.bn_aggr(out=mv, in_=st), [sres], [sres])
            ts("dve", std, mv[:, 1:2], EPS, None, ALU.add, None, [sres], [sres])
            tt("pool", rstd, std, mhalf[:, 0:1], ALU.pow, [sres, "mhalf"], [sres])
            stt("dve", nmr, mv[:, 0:1], -1.0, rstd, ALU.mult, ALU.mult, [sres], [sres])
            ts("dve", dst, src, rstd, nmr, ALU.mult, ALU.add, [src_res, sres], [dst_res])
            tt("dve", dst, dst, g_t, ALU.mult, [dst_res, gb_res], [dst_res])
            tt(beta_eng, dst, dst, b_t, ALU.add, [dst_res, gb_res], [dst_res])

        S.add("pool", lambda e: e.memset(tmp_f[:], 0.0), [], ["tmp_f"])
        S.add("pool", lambda e: e.affine_select(out=ident_f[:], in_=tmp_f[:], pattern=[[-1, 128]], compare_op=ALU.not_equal,
                                                fill=1.0, base=0, channel_multiplier=1), ["tmp_f"], ["ident_f"])
        cp("dve", ident_b[:], ident_f[:], ["ident_f"], ["ident_b"])
        S.add("pool", lambda e: e.memset(tmp_f[:], 1.0), ["tmp_f"], ["tmp_f"])
        cp("dve", ones_b[:], tmp_f[:], ["tmp_f"], ["ones_b"])
        cp("dve", ones_f[:], tmp_f[0:1, :], ["tmp_f"], ["ones_f"])
        S.add("pool", lambda e: e.iota(tmp_f[:], pattern=[[1, 128]], base=0, channel_multiplier=-1,
                                       allow_small_or_imprecise_dtypes=True), ["tmp_f"], ["tmp_f"])
        ts("dve", lstrict[:], tmp_f[:], 0.0, None, ALU.is_gt, None, ["tmp_f"], ["lstrict"])
        S.add("pool", lambda e: e.iota(iota32[:], pattern=[[1, 32]], base=0, channel_multiplier=0,
                                       allow_small_or_imprecise_dtypes=True), [], ["iota32"])
        ts("dve", iotacap[:], iota32[:], float(CAP), None, ALU.mult, None, ["iota32"], ["iotacap"])
        S.add("pool", lambda e: e.iota(iotadiv8[:].rearrange("p (a b) -> p a b", a=4), pattern=[[1, 4], [0, 8]], base=0, channel_multiplier=0,
                                       allow_small_or_imprecise_dtypes=True), [], ["iotadiv8"])
        S.add("pool", lambda e: e.memset(lg8[:], -1e30), [], ["lg8"])
        S.add("pool", lambda e: e.memset(mhalf[:], -0.5), [], ["mhalf"])
        S.add("pool", lambda e: e.iota(trash[:], pattern=[[0, 1]], base=NE * CAP, channel_multiplier=1,
                                       allow_small_or_imprecise_dtypes=True), [], ["trash"])
        S.add("pool", lambda e: e.memset(bg_sb[:], 0.0), [], ["bg"])
        S.add("pool", lambda e: e.memset(mbf[:], 0.0), [], ["mbfz", ("mbf", 0), ("mbf", 1)])
        S.add("pool", lambda e: e.memset(hx, 0.0), [REG], ["hx"] + [("hxq", q_, s_) for q_ in range(8) for s_ in range(2)])
        dma("sp", bmw[:].rearrange("p a c -> p (a c)"), bmw_d.partition_broadcast(128), [], ["bmw"])
        dma("sp", tokinfo[:], tokinfo_d, [], ["tokinfo"])

        def load_layer_weights(l):
            for q4 in range(4):
                dma("pool", w_in_sb[:, :, q4 * 512:(q4 + 1) * 512],
                    w_in_d[l, :, q4 * 512:(q4 + 1) * 512].rearrange("(k p) f -> p k f", p=128), [], [("w_in", q4)])
            for q2 in range(2):
                dma("pool", w_o_sb[:, :, q2 * 512:(q2 + 1) * 512],
                    w_o_d[l, :, q2 * 512:(q2 + 1) * 512].rearrange("(k p) f -> p k f", p=128), [], [("w_o", q2)])
                dma("pool", wg_sb[:, :, q2 * 512:(q2 + 1) * 512],
                    wg_d[l, :, q2 * 512:(q2 + 1) * 512].rearrange("(k p) f -> p k f", p=128), [], [("wg", q2)])
            dma("pool", wp_sb[:], wp_d[l].rearrange("(k p) f -> p k f", p=128), [], ["wp"])
            ts("dve", wp_sb[:].rearrange("p k n -> p (k n)"), wp_sb[:].rearrange("p k n -> p (k n)"), 0.5, None, ALU.mult, None, ["wp"], ["wp"])
            dma("pool", poolw_sb[:], pool_w_d[l].rearrange("g c d -> c g d"), [], ["poolw"])
            dma("pool", bg_sb[0:1, :], bg_d[l], [], ["bg"])
            dma("sp", wr_sb[:], wr_d[l].rearrange("(k p) f -> p k f", p=128), [], ["wr"])
            cp("dve", wr_hi[:], wr_sb[:], ["wr"], ["wrh"])
            tt("dve", wr_lo[:], wr_sb[:], wr_hi[:], ALU.subtract, ["wr", "wrh"], ["wrl"])
            dma("sp", br_sb[:], br_d[l].partition_broadcast(128), [], ["br"])
            dma("sp", cols[:], cols_d[l], [], ["cols"])
            dma("sp", lnA_g[:], lnv_d[l, 0:1, :].partition_broadcast(128), [], ["lnA"])
            dma("sp", lnA_b[:], lnv_d[l, 1:2, :].partition_broadcast(128), [], ["lnA"])

        def load_lnB(l, which):
            if which == 0:
                dma("sp", lnB_g[:], ln0_d[0:1, :].partition_broadcast(128), [], ["lnB"])
                dma("sp", lnB_b[:], ln0_d[1:2, :].partition_broadcast(128), [], ["lnB"])
            else:
                dma("sp", lnB_g[:], lnv_d[l, 2:3, :].partition_broadcast(128), [], ["lnB"])
                dma("sp", lnB_b[:], lnv_d[l, 3:4, :].partition_broadcast(128), [], ["lnB"])

        def phase_a(l):
            nch = KNCH if KNCH else NCHUNK[l]
            base_i = 8 if l == 0 else 136
            src_x = xpad if l == 0 else xcur
            ti_base = 0
            S.add("pool", lambda e: e.memset(carry[:], 0.0), [], ["carry"])

            def xres_reads(i0, n):
                if l == 0:
                    return []
                t0 = (i0 - 8) // 128
                t1 = (i0 + n - 1 - 8) // 128
                return [("xc", t) for t in range(t0, t1 + 1)]

            def load_halo_group(G):
                for q in range(8):
                    j = 8 * G + q
                    if j >= nch:
                        break
                    i0 = base_i + CH * j
                    dma("sp", hx[16 * q:16 * q + 8, :], src_x[i0 - 8:i0, :], [REG, "hxdone"] + xres_reads(i0 - 8, 8), [("hxq", q, 0)])
                    dma("sp", hx[16 * q + 8:16 * q + 16, :], src_x[i0 + CH:i0 + CH + 8, :], [REG, "hxdone"] + xres_reads(i0 + CH, 8), [("hxq", q, 1)])
                hxall = [("hxq", q_, s_) for q_ in range(8) for s_ in range(2)]
                S.add("pool", lambda e: e.tensor_copy(out=hx[0:1, 0:1], in_=hx[0:1, 0:1]), hxall + ["hx"], hxall + ["hx"])
                if l == 0:
                    layernorm(hx, "hx", hx, "hx", lnB_g[:], lnB_b[:], "lnB")
                hb = xbf_t[0]
                cp("dve", hb, hx, ["hx", REG], [("xbf", 0), "hxdone"])
                for k in range(8):
                    tr(PT_bf[:, k * 128:(k + 1) * 128], hb[:, k * 128:(k + 1) * 128], ident_b[:], [("xbf", 0), "ident_b"], ["PT"])
                cp("act", xnTh, PT_bf.rearrange("p (k n) -> p k n", k=8), ["PT"], ["xnTh"])

            def load_chunk_x(j):
                if j >= nch:
                    return
                i0 = base_i + CH * j
                for tt_ in range(2):
                    slot = (2 * j + tt_) % 4
                    dma("sp", xn_t[slot], src_x[i0 + 128 * tt_:i0 + 128 * tt_ + 128, :],
                        [REG] + xres_reads(i0 + 128 * tt_, 128), [("xn", slot)])

            def load_chunk_p(j):
                if j >= nch:
                    return
                for tt_ in range(2):
                    tix = 2 * j + tt_
                    prow = tix * 128 if l == 0 else 128 + tix * 128
                    dma("sp", p_t[tix % 4], ppad[l, prow:prow + 128, :], [REG], [("p", tix % 4)])

            def front(j):
                catT = catT2[j % 2]
                cres = ("catT", j % 2)
                PA, PAr = P67[:, 0:W], "P6"
                PB, PBr = P67[:, 512:512 + W], "P7"
                xb = xnT[j % 2]
                xres = ("xnT", j % 2)
                q = j % 8
                for tt_ in range(2):
                    slot = (2 * j + tt_) % 4
                    if l == 0:
                        layernorm(xn_t[slot], ("xn", slot), xn_t[slot], ("xn", slot), lnB_g[:], lnB_b[:], "lnB", beta_eng="dve")
                    xbf = xbf_t[tt_]
                    cp("act", xbf, xn_t[slot], [("xn", slot), REG], [("xbf", tt_)])
                    for k in range(8):
                        tr(PT_bf[:, k * 128:(k + 1) * 128], xbf[:, k * 128:(k + 1) * 128], ident_b[:],
                           [("xbf", tt_), "ident_b"], ["PT"])
                    cp(os.environ.get("KXNT", "dve"), xb[:, :, 8 + 128 * tt_:8 + 128 * tt_ + 128], PT_bf.rearrange("p (k n) -> p k n", k=8),
                       ["PT"], [xres])
                    yield
                cp("pool", xb[:, :, 0:8], xnTh[:, :, 16 * q:16 * q + 8], ["xnTh"], [xres])
                cp("pool", xb[:, :, CH + 8:CH + 16], xnTh[:, :, 16 * q + 8:16 * q + 16], ["xnTh"], [xres])

                bidx = None
                for bi, (bl, bj, bws) in enumerate(BWIN):
                    if bl == l and bj == j:
                        bidx, ws = bi, bws

                def zmat(fc, pout, pres):
                    for k in range(8):
                        mm(pout, w_in_sb[:, k, fc * 128:(fc + 1) * 128], xb[:, k, :], k == 0, k == 7,
                           [("w_in", fc // 4), xres], [pres])

                for i in range(4):
                    s3 = (i % 2) * 3
                    zh, zgb, zgc = zb[s3], zb[s3 + 1], zb[s3 + 2]
                    rh, rgb, rgc = ("zb", s3), ("zb", s3 + 1), ("zb", s3 + 2)
                    zmat(i, PA, PAr)
                    actf(zh, PA, AF.Identity, [PAr, "cols", REG], [rh], bias=cols[:, i:i + 1])
                    yield
                    zmat(8 + i, PB, PBr)
                    actf(zgc, PB, AF.Identity, [PBr, "cols", REG], [rgc], bias=cols[:, 8 + i:9 + i])
                    yield
                    zmat(4 + i, PA, PAr)
                    actf(zgb, PA, AF.Identity, [PAr, "cols", REG], [rgb], bias=cols[:, 4 + i:5 + i])
                    yield
                    v = mt[0]
                    t1 = mt[1]
                    ce = "pool" if (i % 2 == 1 and os.environ.get("KCONVPOOL", "0") == "1") else "dve"
                    tt(ce, v, zgc, zh, ALU.mult, [rh, rgc, REG], [("mt", 0)])
                    if bidx is not None:
                        tt("dve", v[:, ws:ws + 32], v[:, ws:ws + 32], bmw[:, bidx, 0:32], ALU.mult, [("mt", 0), "bmw"], [("mt", 0)])
                    c0, c1, c2 = 16 + 0 * 4 + i, 16 + 1 * 4 + i, 16 + 2 * 4 + i
                    ts(ce, t1[:, 0:CH], v[:, 7:7 + CH], cols[:, c0:c0 + 1], None, ALU.mult, None, [("mt", 0), "cols"], [("mt", 1)])
                    stt(ce, t1[:, 0:CH], v[:, 8:8 + CH], cols[:, c1:c1 + 1], t1[:, 0:CH], ALU.mult, ALU.add, [("mt", 0), ("mt", 1), "cols"], [("mt", 1)])
                    stt(ce, t1[:, 0:CH], v[:, 9:9 + CH], cols[:, c2:c2 + 1], t1[:, 0:CH], ALU.mult, ALU.add, [("mt", 0), ("mt", 1), "cols"], [("mt", 1)])
                    tt(ce, catT[:, i, :], zgb[:, 8:8 + CH], t1[:, 0:CH], ALU.mult, [rgb, ("mt", 1), REG], [cres])
                    yield
                for g in range(4):
                    wdt = POOL_W[g]
                    zu = zb[g % 2 * 3]
                    ru = ("zb", g % 2 * 3)
                    pz, pzr = (PA, PAr) if g % 2 == 0 else (PB, PBr)
                    py, pyr = (PB[:, 0:CH], PBr) if g % 2 == 0 else (PA[:, 0:CH], PAr)
                    zmat(12 + g, pz, pzr)
                    ts("dve", zu, pz, cols[:, 12 + g:13 + g], None, ALU.add, None, [pzr, "cols", REG], [ru])
                    yield
                    if bidx is not None:
                        tt("dve", zu[:, ws:ws + 32], zu[:, ws:ws + 32], bmw[:, bidx, 0:32], ALU.mult, [ru, "bmw"], [ru])
                    a1, a2, a3 = mt[2], mt[3], mt[4]
                    ssum = mt[5]
                    if wdt == 2:
                        tt("dve", ssum[:, 8:8 + CH], zu[:, 7:7 + CH], zu[:, 8:8 + CH], ALU.add, [ru, REG], [("mt", 5)])
                    else:
                        tt("dve", a1[:, 0:W - 1], zu[:, 0:W - 1], zu[:, 1:W], ALU.add, [ru, REG], [("mt", 2)])
                        if wdt == 4:
                            tt("dve", ssum[:, 8:8 + CH], a1[:, 6:6 + CH], a1[:, 8:8 + CH], ALU.add, [("mt", 2)], [("mt", 5)])
                        else:
                            tt("dve", a2[:, 0:W - 3], a1[:, 0:W - 3], a1[:, 2:W - 1], ALU.add, [("mt", 2)], [("mt", 3)])
                            if wdt == 8:
                                tt("dve", ssum[:, 8:8 + CH], a2[:, 4:4 + CH], a2[:, 8:8 + CH], ALU.add, [("mt", 3)], [("mt", 5)])
                            else:
                                tt("dve", a3[:, 0:W - 7], a2[:, 0:W - 7], a2[:, 4:W - 3], ALU.add, [("mt", 3)], [("mt", 4)])
                                tt("dve", ssum[:, 8:8 + CH], a3[:, 0:CH], a3[:, 8:8 + CH], ALU.add, [("mt", 4)], [("mt", 5)])
                    pl = pooledT[g % 2]
                    plr = ("pooledT", g % 2)
                    stt("dve", pl, ssum[:, 8:8 + CH], 1.0 / wdt, zu[:, 8:8 + CH], ALU.mult, ALU.subtract, [("mt", 5), ru, REG], [plr])
                    if bidx is not None:
                        lo = max(ws, 8)
                        hi = min(ws + 32, 8 + CH)
                        tmpw = mt[2]
                        tt("dve", tmpw[:, lo:hi], ssum[:, lo:hi], bmw[:, bidx, 32 * (1 + g) + (lo - ws):32 * (1 + g) + (hi - ws)],
                           ALU.mult, [("mt", 5), "bmw", ("mt", 2)], [("mt", 2)])
                        tt("dve", pl[:, lo - 8:hi - 8], tmpw[:, lo:hi], zu[:, lo:hi], ALU.subtract, [("mt", 2), ru, plr], [plr])
                    mm(py, poolw_sb[:, g, :], pl, True, True, ["poolw", plr], [pyr])
                    actf(catT[:, 4 + g, :], py, AF.Identity, [pyr, "cols", REG], [cres], scale=cols[:, 28 + g:29 + g])
                    yield

            def back(j, tiles=(0, 1)):
                catT = catT2[j % 2]
                cres = ("catT", j % 2)
                for tt_ in tiles:
                    tix = 2 * j + tt_
                    slot = (2 * j + tt_) % 4
                    b2 = tix % 2
                    for half in range(2):
                        for k in range(8):
                            mm(P45[:, half * 512:(half + 1) * 512], catT[:, k, tt_ * 128:(tt_ + 1) * 128],
                               w_o_sb[:, k, half * 512:(half + 1) * 512], k == 0, k == 7, [cres, ("w_o", half)], ["P45"])
                    MARKS.setdefault("mix_done", len(S.ops))
                    r = r_t[0]
                    stt("dve", r, xn_t[slot], ALPHA, P45[:, :], ALU.mult, ALU.add, [("xn", slot), "P45", REG], [("r", 0)])
                    if tt_ == 1:
                        load_chunk_x(j + 2)
                    yield
                    x1 = x1_t[b2]
                    layernorm(r, ("r", 0), x1, ("x1", b2), lnA_g[:], lnA_b[:], "lnA", beta_eng="dve")
                    yield
                    MARKS.setdefault("ln1_done", len(S.ops))
                    cp("act", x1bf[b2], x1, [("x1", b2)], [("x1bf", b2)])
                    for k in range(8):
                        tr(P23[:, k * 128:(k + 1) * 128], x1[:, k * 128:(k + 1) * 128], ident_f[:], [("x1", b2), "ident_f"], ["P2", "P3"])
                    yield
                    cp("dve", x1T32, P23.rearrange("p (k n) -> p k n", k=8), ["P2", "P3", REG], ["x1T32"])
                    if os.environ.get("KX1TB", "dve") == "act":
                        for hb_ in range(2):
                            cp("act", x1Tb[b2][:, 4 * hb_:4 * hb_ + 4, :], P23[:, 512 * hb_:512 * hb_ + 512].rearrange("p (k n) -> p k n", k=4),
                               ["P2", "P3", REG], [("x1Tb", b2)])
                    else:
                        cp("dve", x1Tb[b2], P23.rearrange("p (k n) -> p k n", k=8), ["P2", "P3", REG], [("x1Tb", b2)])
                    MARKS.setdefault("x1T_done", len(S.ops))
                    yield
                    PL = P01[:, 512:576]
                    if os.environ.get("KROUTER", "f32") == "f32":
                        for k in range(8):
                            mm(PL, x1T32[:, k, :], wr_sb[:, k, :], k == 0, k == 7, ["x1T32", "wr"], ["P1"])
                    else:
                        tt("dve", x1Tlo, x1T32, x1Tb[b2], ALU.subtract, ["x1T32", ("x1Tb", b2)], ["x1Tlo"])
                        for k in range(8):
                            mm(PL, x1Tb[b2][:, k, :], wr_hi[:, k, :], k == 0, False, [("x1Tb", b2), "wrh"], ["P1"])
                            mm(PL, x1Tb[b2][:, k, :], wr_lo[:, k, :], False, False, [("x1Tb", b2), "wrl"], ["P1"])
                            mm(PL, x1Tlo[:, k, :], wr_hi[:, k, :], False, k == 7, ["x1Tlo", "wrh"], ["P1"])
                    MARKS.setdefault("logits_done", len(S.ops))
                    yield
                    yield from route(l, tix, PL)
                    MARKS.setdefault("route_done", len(S.ops))
                    for kk in range(2):
                        if STAGE != "a0":
                            scatter(xs_d[l], dest_i[:, tix, kk:kk + 1], x1bf[b2], [("x1bf", b2), ("dest", tix)], [("xsw", l, tix, kk)])
                    yield
                    for half in range(2):
                        PG = P45[:, half * 512:(half + 1) * 512]
                        for k in range(8):
                            mm(PG, x1Tb[b2][:, k, :], wg_sb[:, k, half * 512:(half + 1) * 512], k == 0, False, [("x1Tb", b2), ("wg", half)], ["P45"])
                        mm(PG, ones_b[:], bg_sb[:, half * 512:(half + 1) * 512], False, True, ["ones_b", "bg"], ["P45"])
                    gt = gate_t[b2]
                    for half in range(2):
                        actf(gt[:, half * 512:(half + 1) * 512], P45[:, half * 512:(half + 1) * 512], AF.Tanh, ["P45", REG], [("gate", b2)], scale=0.5)
                    MARKS.setdefault("gate_done", len(S.ops))
                    yield
                    ptile = p_t[tix % 4]
                    PP = P01[:, 704:960]
                    for k in range(2):
                        tr(PP[:, k * 128:(k + 1) * 128], ptile[:, k * 128:(k + 1) * 128], ident_f[:], [("p", tix % 4), "ident_f"], ["P1"])
                    cp("dve", pT[b2], PP.rearrange("p (k n) -> p k n", k=2), ["P1", REG], [("pT", b2)])
                    for half in range(2):
                        for k in range(2):
                            mm(P23[:, half * 512:(half + 1) * 512], pT[b2][:, k, :], wp_sb[:, k, half * 512:(half + 1) * 512],
                               k == 0, k == 1, [("pT", b2), "wp"], ["P2", "P3"])
                    yield
                    stt("dve", gt, gt, 1.0, P23[:, :], ALU.add, ALU.mult, [("gate", b2), "P2", "P3"], [("gate", b2)])
                    stt("dve", gt, x1, ALPHA, gt, ALU.mult, ALU.add, [("x1", b2), ("gate", b2)], [("gate", b2)])
                    dma("sp", r2p_d[l][tix * 128:(tix + 1) * 128, :], gt, [("gate", b2)], [("r2p", l, tix)])
                    yield
                if 1 in tiles:
                    load_chunk_p(j + 2)

            def run_gens(gens):
                gens = list(gens)
                while gens:
                    for g_ in list(gens):
                        try:
                            next(g_)
                        except StopIteration:
                            gens.remove(g_)

            load_chunk_x(0)
            load_chunk_p(0)
            load_chunk_x(1)
            load_chunk_p(1)
            load_halo_group(0)
            run_gens([front(0)])
            for j in range(nch):
                if os.environ.get("KSPLIT", "0") == "1":
                    gens = [back(j, (0,)), back(j, (1,))]
                else:
                    gens = [back(j)]
                if j + 1 < nch:
                    if (j + 1) % 8 == 0:
                        load_halo_group((j + 1) // 8)
                    if os.environ.get("KORDER", "back") == "front":
                        gens.insert(0, front(j + 1))
                    else:
                        gens.append(front(j + 1))
                run_gens(gens)

        def route(l, tix, PL):
            k = tix % 2
            rs = ("rt", k)
            A = rt[:, k, :]
            U = rtu[:, k, :]
            lgt = A[:, 0:36]
            mx8 = A[:, 40:48]
            negmax = A[:, 48:49]
            ex4 = A[:, 52:56]
            sume = A[:, 56:57]
            gw = A[:, 57:58]
            gself = A[:, 58:59]
            gsel8 = A[:, 59:60]
            oh4 = A[:, 60:64]
            el = A[:, 64:72]
            tv8 = A[:, 72:80]
            dd = A[:, 80:81]
            ee = A[:, 81:82]
            den = A[:, 82:83]
            w0 = A[:, 83:84]
            w1 = A[:, 84:85]
            tif = A[:, 86:88]
            eid = A[:, 88:90]
            oh0 = A[:, 96:128]
            oh1 = A[:, 128:160]
            rank = A[:, 160:192]
            tmp32 = A[:, 192:224]
            destf = A[:, 224:226]
            lg8k = A[:, 232:240]
            gi8 = U[:, 0:8]
            ti8 = U[:, 8:16]
            tt("dve", lgt, PL[:, 0:36], br_sb[:, 0:36], ALU.add, ["P1", "br"], [rs])
            cp("dve", lg8k, lg8[:], [rs, "lg8"], [rs])
            cp("dve", lg8k[:, 0:4], lgt[:, 0:4], [rs], [rs])
            S.add("dve", lambda e: e.max(out=mx8, in_=lg8k), [rs], [rs])
            S.add("dve", lambda e: e.max_index(out=gi8, in_max=mx8, in_values=lg8k), [rs], [rs])
            ts("dve", negmax, mx8[:, 0:1], -1.0, None, ALU.mult, None, [rs], [rs])
            actf(ex4, lgt[:, 0:4], AF.Exp, [rs], [rs], bias=negmax, scale=1.0, accum=sume)
            S.add("dve", lambda e: e.reciprocal(out=gw, in_=sume), [rs], [rs])
            yield
            cp("dve", gself, gi8[:, 0:1], [rs], [rs])
            pen = oh0
            elm = oh1
            ts("dve", pen, iotadiv8[:], gself, -1e30, ALU.not_equal, ALU.mult, [rs, "iotadiv8"], [rs])
            tt("dve", elm, lgt[:, 4:36], pen, ALU.add, [rs], [rs])
            S.add("dve", lambda e: e.max(out=tv8, in_=elm), [rs], [rs])
            S.add("dve", lambda e: e.max_index(out=ti8, in_max=tv8, in_values=elm), [rs], [rs])
            yield
            tt("dve", dd, tv8[:, 1:2], tv8[:, 0:1], ALU.subtract, [rs], [rs])
            actf(ee, dd, AF.Exp, [rs], [rs])
            ts("dve", den, ee, 1.0, None, ALU.add, None, [rs], [rs])
            S.add("dve", lambda e: e.reciprocal(out=w0, in_=den), [rs], [rs])
            tt("dve", w1, ee, w0, ALU.mult, [rs], [rs])
            ts("dve", wts[:, tix, :], A[:, 83:85], gw, None, ALU.mult, None, [rs], [("wts", tix)])
            cp("dve", eid, ti8[:, 0:2], [rs], [rs])
            tcol = (l * NT[0] + tix) * 2
            yield
            maybe_invalid = (l == 0 and tix in (0, NT[0] - 1))
            if maybe_invalid:
                ts("dve", oh0, iota32[:], eid[:, 0:1], tokinfo[:, tcol:tcol + 1], ALU.is_equal, ALU.mult, [rs, "iota32", "tokinfo"], [rs])
                ts("dve", oh1, iota32[:], eid[:, 1:2], tokinfo[:, tcol:tcol + 1], ALU.is_equal, ALU.mult, [rs, "iota32", "tokinfo"], [rs])
            else:
                ts("dve", oh0, iota32[:], eid[:, 0:1], None, ALU.is_equal, None, [rs, "iota32"], [rs])
                ts("dve", oh1, iota32[:], eid[:, 1:2], None, ALU.is_equal, None, [rs, "iota32"], [rs])
            mres = ("mbf", k)
            tt("dve", mbf[:, k, 0:32], oh0, oh1, ALU.add, [rs, "mbfz"], [mres])
            PR = P01[:, 576:704]
            mm(PR[:, 0:64], lstrict[:], mbf[:, k, :], True, True, ["lstrict", mres], ["P1"])
            mm(PR[:, 64:128], ones_b[:], mbf[:, k, :], True, True, ["ones_b", mres], ["P1"])
            yield
            tt("dve", rank, PR[:, 0:32], carry[:], ALU.add, ["P1", "carry", rs], [rs])
            tt("dve", carry[:], PR[:, 64:96], carry[:], ALU.add, ["P1", "carry"], ["carry"])
            rsel = A[:, 240:242]
            ovf = A[:, 242:244]
            keep = A[:, 244:246]
            tt("dve", tmp32, rank, oh0, ALU.mult, [rs], [rs])
            S.add("dve", lambda e: e.reduce_sum(out=rsel[:, 0:1], in_=tmp32, axis=AX.X), [rs], [rs])
            tt("dve", tmp32, rank, oh1, ALU.mult, [rs], [rs])
            S.add("dve", lambda e: e.reduce_sum(out=rsel[:, 1:2], in_=tmp32, axis=AX.X), [rs], [rs])
            yield
            stt("dve", destf, eid, float(CAP), rsel, ALU.mult, ALU.add, [rs], [rs])
            ts("dve", ovf, rsel, float(CAP) - 0.5, None, ALU.is_gt, None, [rs], [rs])
            ts("dve", keep, ovf, -1.0, 1.0, ALU.mult, ALU.add, [rs], [rs])
            if maybe_invalid:
                ts("dve", destf, destf, tokinfo[:, tcol:tcol + 1], None, ALU.mult, None, [rs, "tokinfo"], [rs])
            tt("dve", destf, destf, keep, ALU.mult, [rs], [rs])
            stt("dve", destf, ovf, trash[:, 0:1], destf, ALU.mult, ALU.add, [rs, "trash"], [rs])
            if maybe_invalid:
                ts("dve", destf, destf, tokinfo[:, tcol + 1:tcol + 2], None, ALU.add, None, [rs, "tokinfo"], [rs])
            tt("dve", wts[:, tix, :], wts[:, tix, :], keep, ALU.mult, [rs, ("wts", tix)], [("wts", tix)])
            cp("dve", dest_i[:, tix, :], destf, [rs], [("dest", tix)])
            if DEBUG:
                cp("dve", A[:, 226:228], destf, [rs], [rs])
                cp("dve", A[:, 228:230], wts[:, tix, :], [rs, ("wts", tix)], [rs])
                dma("sp", dbg_route[l, :, tix * 4:tix * 4 + 4], A[:, 226:230], [rs], [("dbgr", l, tix)])

        def phase_b(l):
            nt = NT[l]
            xs_reads = [("xsw", l, t, kk) for t in range(nt) for kk in range(2)]

            def load_e(e):
                b = e % 2
                first = [REG] if e < 2 else []
                wres = ("ew", b)
                for hh in range(2):
                    dma("pool", w1_sb[b][:, 4 * hh:4 * hh + 4, :],
                        w1_d[l, e].rearrange("(p k) f -> p k f", k=8)[:, 4 * hh:4 * hh + 4, :], [], [("ew", b, 1, hh)] + (first if hh == 0 else []))
                    dma("pool", w3_sb[b][:, 4 * hh:4 * hh + 4, :],
                        w3_d[l, e].rearrange("(p k) f -> p k f", k=8)[:, 4 * hh:4 * hh + 4, :], [], [("ew", b, 3, hh)])
                    dma("pool", w2_sb[b][:, 2 * hh:2 * hh + 2, :],
                        w2_d[l, e].rearrange("(k p) f -> p k f", p=128)[:, 2 * hh:2 * hh + 2, :], [], [("ew", b, 2, hh)])
                dma("sp", xs_sb[b], xs_d[l][e * CAP:(e + 1) * CAP, :].rearrange("(j p) d -> p j d", p=128),
                    xs_reads, [("xs", b)] + first)

            def comp_e(e):
                b = e % 2
                wres = ("ew", b)
                for j in range(3):
                    for k in range(8):
                        tr(PT_bf[:, k * 128:(k + 1) * 128], xs_sb[b][:, j, :].rearrange("p (q k) -> p k q", k=8)[:, k, :], ident_b[:],
                           [("xs", b), "ident_b"], ["PT"])
                    eng = "act" if j % 2 == 0 else "dve"
                    cp(eng, xsT[:, :, j * 128:(j + 1) * 128], PT_bf.rearrange("p (k n) -> p k n", k=8), ["PT", REG], ["xsT"])
                for m in range(4):
                    p1 = P23[:, 0:CAP] if m % 2 == 0 else P23[:, 512:512 + CAP]
                    p1r = "P2" if m % 2 == 0 else "P3"
                    p3 = P45[:, 0:CAP] if m % 2 == 0 else P45[:, 512:512 + CAP]
                    p3r = "P4" if m % 2 == 0 else "P5"
                    for k in range(8):
                        mm(p1, w1_sb[b][:, k, m * 128:(m + 1) * 128], xsT[:, k, :], k == 0, k == 7, [("ew", b, 1, k // 4), "xsT"], [p1r])
                    for k in range(8):
                        mm(p3, w3_sb[b][:, k, m * 128:(m + 1) * 128], xsT[:, k, :], k == 0, k == 7, [("ew", b, 3, k // 4), "xsT"], [p3r])
                    sl = silu_t[m % 2]
                    actf(sl, p1, AF.Silu, [p1r, REG], [("silu", m % 2)])
                    tt("dve", hT[:, m, :], sl, p3, ALU.mult, [("silu", m % 2), p3r, REG], ["hT"])
                for j in range(3):
                    for half in range(2):
                        py = P67[:, half * 512:(half + 1) * 512]
                        pyr = "P6" if half == 0 else "P7"
                        for m in range(4):
                            mm(py, hT[:, m, j * 128:(j + 1) * 128], w2_sb[b][:, m, half * 512:(half + 1) * 512],
                               m == 0, m == 3, ["hT", ("ew", b, 2, m // 2)], [pyr])
                        eng = "act" if half == 0 else "dve"
                        cp(eng, ys_sb[:, j, half * 512:(half + 1) * 512], py, [pyr, REG], ["ys_sb"])
                dma("sp", ys_d[l][e * CAP:(e + 1) * CAP, :].rearrange("(j p) d -> p j d", p=128), ys_sb, ["ys_sb"], [("ysw", l, e)])

            load_e(0)
            for e in range(NE):
                if e + 1 < NE:
                    load_e(e + 1)
                comp_e(e)

        def phase_c(l):
            nt = NT[l]
            ys_reads = [("ysw", l, e) for e in range(NE)]
            stores = []

            def loads(t):
                b = t % NCB
                for kk, buf in ((0, cy0), (1, cy1)):
                    gather(buf[b], ys_d[l], dest_i[:, t, kk:kk + 1], ys_reads + [("dest", t), REG], [("cy", kk, b)])
                dma("sp", cr[b], r2p_d[l][t * 128:(t + 1) * 128, :], [("r2p", l, t), REG], [("cr", b)])

            def comp(t):
                b = t % NCB
                stt("dve", cr[b], cy0[b], wts[:, t, 0:1], cr[b], ALU.mult, ALU.add, [("cy", 0, b), ("cr", b), ("wts", t)], [("cr", b)])
                stt("dve", cr[b], cy1[b], wts[:, t, 1:2], cr[b], ALU.mult, ALU.add, [("cy", 1, b), ("cr", b), ("wts", t)], [("cr", b)])
                layernorm(cr[b], ("cr", b), cy0[b], ("cy", 0, b), lnB_g[:], lnB_b[:], "lnB", beta_eng="dve")
                if l == 0:
                    stores.append(dma("sp", xcur[8 + t * 128:8 + (t + 1) * 128, :], cy0[b], [("cy", 0, b)], [("xc", t)]))
                else:
                    stores.append(dma("sp", out_d[t * 128:(t + 1) * 128, :], cy0[b], [("cy", 0, b)], [("out", t)]))

            for b_ in range(NCB):
                S.add("pool", (lambda ap: (lambda e: e.memset(ap, 0.0)))(cy0[b_]), [], [("cy", 0, b_)])
                S.add("pool", (lambda ap: (lambda e: e.memset(ap, 0.0)))(cy1[b_]), [], [("cy", 1, b_)])
            dma("sp", ys_d[l][NE * CAP:NE * CAP + 128, :], cy0[0], [("cy", 0, 0)], [("ysw", l, "trash")])
            ys_reads.append(("ysw", l, "trash"))
            for t in range(min(NCB - 1, nt)):
                loads(t)
            for t in range(nt):
                if t + NCB - 1 < nt:
                    loads(t + NCB - 1)
                comp(t)
            return stores

        final = []
        load_layer_weights(0)
        load_lnB(0, 0)
        for l in range(DEPTH):
            if STAGE == "w":
                break
            phase_a(l)
            if STAGE in ("a0", "a0s"):
                break
            load_lnB(l, 1)
            if l + 1 < DEPTH:
                load_layer_weights(l + 1)
            S.barrier()
            phase_b(l)
            if STAGE == "b0":
                break
            S.barrier()
            final = phase_c(l)
            S.barrier()
            if STAGE == "c0":
                break
        dbg = [i for i, op in enumerate(S.ops) if op.dma] if DEBUG else []
        S.emit(nc, final_wait_ops=list(final) + dbg)
    return nc


def _boundary_consts(own_start):
    bm = np.zeros((4, 5, 32), np.float32)
    for bi, (l, j, ws) in enumerate(BWIN):
        for c in range(32):
            col = ws + c
            if l == 0:
                tok = own_start - 136 + CH * j + col
            else:
                tok = own_start + CH * j + col - 8
            valid = 0 <= tok < SEQ
            bm[bi, 0, c] = 1.0 if valid else 0.0
            for g, wdt in enumerate(POOL_W):
                left = wdt // 2
                right = wdt - 1 - left
                cnt = min(tok + right + 1, SEQ) - max(tok - left, 0)
                bm[bi, 1 + g, c] = (1.0 / cnt) if (valid and cnt > 0) else 0.0
    return bm.reshape(1, 640)


_NC_CACHE = {}


def kernel(x, p, ln0_g, ln0_b, w_in, b_in, conv_w, pool_w, pool_scale, w_o,
           ln1_g, ln1_b, w_router_group, b_router_group, w_router_expert,
           b_router_expert, w1, w3, w2, w_ple_gate, b_ple_gate, w_ple_proj,
           ln2_g, ln2_b):
    f32 = lambda a: np.ascontiguousarray(np.asarray(a), dtype=np.float32)
    x = f32(x); p = f32(p)
    L = DEPTH
    wr = np.concatenate([f32(w_router_group), f32(w_router_expert).transpose(0, 2, 1, 3).reshape(L, D, 32),
                         np.zeros((L, D, 28), np.float32)], axis=2)
    br = np.concatenate([f32(b_router_group), f32(b_router_expert).reshape(L, 32),
                         np.zeros((L, 28), np.float32)], axis=1).reshape(L, 1, 64)
    colsv = np.zeros((L, 128, 32), np.float32)
    colsv[:, :, 0:16] = f32(b_in).reshape(L, 16, 128).transpose(0, 2, 1)
    cw = f32(conv_w).reshape(L, 3, 4, 128)
    colsv[:, :, 16:28] = cw.transpose(0, 3, 1, 2).reshape(L, 128, 12)
    colsv[:, :, 28:32] = f32(pool_scale).reshape(L, 4, 128).transpose(0, 2, 1)
    ln0 = np.stack([f32(ln0_g), f32(ln0_b)], axis=0)
    lnv = np.stack([f32(ln1_g), f32(ln1_b), f32(ln2_g), f32(ln2_b)], axis=1)
    shared = {
        "ln0": ln0, "lnv": np.ascontiguousarray(lnv), "cols": colsv,
        "w_in": f32(w_in), "pool_w": f32(pool_w), "w_o": f32(w_o), "wr": np.ascontiguousarray(wr),
        "br": np.ascontiguousarray(br), "w1": f32(w1)[:, :NE_DECL], "w3": f32(w3)[:, :NE_DECL], "w2": f32(w2)[:, :NE_DECL],
        "w_ple_gate": f32(w_ple_gate), "b_ple_gate": f32(b_ple_gate).reshape(L, 1, D),
        "w_ple_proj": f32(w_ple_proj),
    }
    in_maps = []
    for c in range(NCORES):
        b, h = c // 2, c % 2
        own = h * OWN
        xp = np.zeros((XROWS, D), np.float32)
        t0 = own - 136
        lo, hi = max(t0, 0), min(t0 + XROWS, SEQ)
        xp[lo - t0:hi - t0] = x[b, lo:hi]
        pp = np.zeros((L, NT[0] * 128, DPLE), np.float32)
        t0p = own - 128
        lo, hi = max(t0p, 0), min(t0p + NT[0] * 128, SEQ)
        pp[:, lo - t0p:hi - t0p] = p[:, b, lo:hi]
        m = dict(shared)
        m["xpad"] = xp
        m["ppad"] = pp
        m["bmw"] = _boundary_consts(own)
        ti = np.zeros((128, DEPTH, NT[0], 2), np.float32)
        ti[:, 1, :, 0] = 1.0
        lidx = np.arange(NT[0])[None, :] * 128 + np.arange(128)[:, None]
        tok = own - 128 + lidx
        v0 = ((tok >= 0) & (tok < SEQ)).astype(np.float32)
        ti[:, 0, :, 0] = v0
        ti[:, 0, :, 1] = (1.0 - v0) * (float(NE * CAP) + np.arange(128, dtype=np.float32)[:, None])
        m["tokinfo"] = ti.reshape(128, DEPTH * NT[0] * 2)
        in_maps.append(m)
    if "nc" not in _NC_CACHE:
        _NC_CACHE["nc"] = build_nc()
    nc = _NC_CACHE["nc"]
    res = run_bass_kernel_spmd(nc, in_maps, core_ids=list(range(NCORES)))
    out = np.zeros((4, SEQ, D), np.float32)
    for c in range(NCORES):
        b, h = c // 2, c % 2
        out[b, h * OWN:(h + 1) * OWN] = res.results[c]["out"]
    kernel.last = res
    return out
```

```python
from contextlib import ExitStack

import numpy as np
import concourse.bass as bass
import concourse.mybir as mybir
from concourse.bass_utils import run_bass_kernel_spmd

F32 = mybir.dt.float32
BF16 = mybir.dt.bfloat16
I32 = mybir.dt.int32
U32 = mybir.dt.uint32
AF = mybir.ActivationFunctionType
ALU = mybir.AluOpType
AX = mybir.AxisListType

NCORES = 8
D = 1024
DIN = 2048
NE = 32
DE = 512
DPLE = 256
SEQ = 8192
OWN = 4096
CAP = 384
CH = 256
W = CH + 16
DEPTH = 2
ALPHA = float((2 * DEPTH) ** 0.25)
EPS = 1e-5
NT = (34, 32)
NCHUNK = (17, 16)
XROWS = 4368
POOL_W = (2, 4, 8, 16)
BWIN = ((0, 0, 120), (0, 16, 120), (1, 0, 0), (1, 15, 240))
import os
DEBUG = bool(int(os.environ.get("KDEBUG", "0")))
STAGE = os.environ.get("KSTAGE", "all")
KNCH = int(os.environ.get("KNCH", "0"))
KMAXOPS = int(os.environ.get("KMAXOPS", "0"))
MARKS = {}
NE_DECL = 1 if STAGE in ("w", "a0", "a0s") else 32

ENG_ATTR = {"pe": "tensor", "act": "scalar", "dve": "vector", "pool": "gpsimd", "sp": "sync"}
NDMA_SEM = {"sp": 16, "act": 4, "pool": 16}


class Op:
    __slots__ = ("eng", "fn", "deps", "dma", "idx", "sig", "sem", "val", "extra")

    def __init__(self, eng, fn, deps, dma, idx):
        self.eng, self.fn, self.deps, self.dma, self.idx = eng, fn, deps, dma, idx
        self.sig = False
        self.sem = None
        self.val = 0
        self.extra = None


class Sched:
    def __init__(self):
        self.ops = []
        self.lastw = {}
        self.rd_eng = {}
        self.rd_dma = {}
        self.dma_since = []
        self.bar_set = set()
        self.bar_pending = set()

    def barrier(self):
        last = {}
        for op in self.ops:
            last[op.eng] = op.idx
        self.bar_set = set(last.values()) | set(self.dma_since)
        self.dma_since = []
        self.bar_pending = set(ENG_ATTR)

    def add(self, eng, fn, reads=(), writes=(), dma=False):
        i = len(self.ops)
        if KMAXOPS and i >= KMAXOPS:
            return i - 1
        deps = set()
        if eng in self.bar_pending:
            deps |= self.bar_set
            self.bar_pending.discard(eng)
        if dma:
            self.dma_since.append(i)
        for r in reads:
            w = self.lastw.get(r)
            if w is not None:
                deps.add(w)
        for r in writes:
            w = self.lastw.get(r)
            if w is not None:
                deps.add(w)
            for rd in self.rd_eng.get(r, {}).values():
                deps.add(rd)
            for rd in self.rd_dma.get(r, ()):
                deps.add(rd)
        op = Op(eng, fn, deps, dma, i)
        self.ops.append(op)
        for r in reads:
            if dma:
                self.rd_dma.setdefault(r, []).append(i)
            else:
                self.rd_eng.setdefault(r, {})[eng] = i
        for r in writes:
            self.lastw[r] = i
            self.rd_eng[r] = {}
            self.rd_dma[r] = []
        return i

    def emit(self, nc, final_wait_ops=()):
        ops = self.ops
        for op in ops:
            if op.eng == "pe" and not op.dma:
                op.deps = {d for d in op.deps if not (ops[d].eng == "pe" and not ops[d].dma)}
        last_per_eng = {}
        for op in ops:
            last_per_eng[op.eng] = op.idx
        fence = Op("sp", None, set(final_wait_ops) | set(last_per_eng.values()), False, len(ops))
        ops = ops + [fence]
        for op in ops:
            for d in op.deps:
                ops[d].sig = True
        with ExitStack() as es:
            esem = {e: es.enter_context(nc.semaphore("prog_" + e)) for e in ENG_ATTR}
            dsem = {q: [es.enter_context(nc.semaphore(f"dma_{q}_{k}")) for k in range(n)]
                    for q, n in NDMA_SEM.items()}
            cnt = {e: 0 for e in ENG_ATTR}
            ndma = {q: 0 for q in NDMA_SEM}
            slot_prev = {q: [None] * n for q, n in NDMA_SEM.items()}
            for op in ops:
                if op.dma:
                    q = op.eng
                    k = ndma[q]
                    ndma[q] += 1
                    s = k % NDMA_SEM[q]
                    op.sem = dsem[q][s]
                    op.val = 16 * (k // NDMA_SEM[q] + 1)
                    op.extra = slot_prev[q][s]
                    slot_prev[q][s] = op.idx
                elif op.sig:
                    cnt[op.eng] += 1
                    op.sem = esem[op.eng]
                    op.val = cnt[op.eng]
            per_eng = {e: [op for op in ops if op.eng == e] for e in ENG_ATTR}
            block = es.enter_context(nc.Block())

            def make(e):
                def body(eng):
                    known = {}
                    for op in per_eng[e]:
                        need = {}
                        deps = set(op.deps)
                        if op.extra is not None:
                            deps.add(op.extra)
                        for d in deps:
                            dop = ops[d]
                            key = id(dop.sem)
                            if key not in need or need[key][1] < dop.val:
                                need[key] = (dop.sem, dop.val)
                        for key, (sem, val) in need.items():
                            if known.get(key, 0) >= val:
                                continue
                            eng.wait_ge(sem, val)
                            known[key] = val
                        if op.fn is None:
                            continue
                        inst = op.fn(eng)
                        if op.dma:
                            inst.then_inc(op.sem, 16)
                        elif op.sig:
                            inst.then_inc(op.sem, 1)
                return body

            for e, attr in ENG_ATTR.items():
                getattr(block, attr)(make(e))


def build_nc():
    nc = bass.Bass("TRN2", target_bir_lowering=False)
    S = Sched()

    def din(name, shape, dt=F32):
        return nc.dram_tensor(name, list(shape), dt, kind="ExternalInput").ap()

    def dscr(name, shape, dt=F32):
        kind = "ExternalOutput" if (DEBUG and name in ("r2p0", "dbg_route", "xcur")) else "Internal"
        return nc.dram_tensor(name, list(shape), dt, kind=kind).ap()

    xpad = din("xpad", [XROWS, D])
    ppad = din("ppad", [DEPTH, NT[0] * 128, DPLE])
    bmw_d = din("bmw", [1, 640])
    tokinfo_d = din("tokinfo", [128, DEPTH * NT[0] * 2])
    ln0_d = din("ln0", [2, D])
    lnv_d = din("lnv", [DEPTH, 4, D])
    cols_d = din("cols", [DEPTH, 128, 32])
    w_in_d = din("w_in", [DEPTH, D, DIN])
    pool_w_d = din("pool_w", [DEPTH, 4, 128, 128])
    w_o_d = din("w_o", [DEPTH, D, D])
    wr_d = din("wr", [DEPTH, D, 64])
    br_d = din("br", [DEPTH, 1, 64])
    w1_d = din("w1", [DEPTH, NE_DECL, D, DE])
    w3_d = din("w3", [DEPTH, NE_DECL, D, DE])
    w2_d = din("w2", [DEPTH, NE_DECL, DE, D])
    wg_d = din("w_ple_gate", [DEPTH, D, D])
    bg_d = din("b_ple_gate", [DEPTH, 1, D])
    wp_d = din("w_ple_proj", [DEPTH, DPLE, D])
    out_d = nc.dram_tensor("out", [OWN, D], F32, kind="ExternalOutput").ap()

    xcur = dscr("xcur", [XROWS, D])
    r2p_d = [dscr(f"r2p{l}", [NT[0] * 128, D]) for l in range(DEPTH)]
    xs_d = [dscr(f"xs{l}", [NE * CAP + 128, D], BF16) for l in range(DEPTH)]
    ys_d = [dscr(f"ys{l}", [NE * CAP + 128, D]) for l in range(DEPTH)]
    if DEBUG:
        dbg_route = dscr("dbg_route", [DEPTH, 128, NT[0] * 4])

    es = ExitStack()
    with es:
        def sb(name, shape, dt):
            return es.enter_context(nc.sbuf_tensor("s_" + name, list(shape), dt))

        w_in_sb = sb("w_in_sb", [128, 8, DIN], BF16)
        w_o_sb = sb("w_o_sb", [128, 8, D], BF16)
        wg_sb = sb("wg_sb", [128, 8, D], BF16)
        wp_sb = sb("wp_sb", [128, 2, D], BF16)
        poolw_sb = sb("poolw_sb", [128, 4, 128], BF16)
        wr_sb = sb("wr_sb", [128, 8, 64], F32)
        wr_hi = sb("wr_hi", [128, 8, 64], BF16)
        wr_lo = sb("wr_lo", [128, 8, 64], BF16)
        tokinfo = sb("tokinfo", [128, DEPTH * NT[0] * 2], F32)
        br_sb = sb("br_sb", [128, 64], F32)
        bg_sb = sb("bg_sb", [128, D], BF16)
        cols = sb("cols", [128, 32], F32)
        lnA_g = sb("lnA_g", [128, D], F32)
        lnA_b = sb("lnA_b", [128, D], F32)
        lnB_g = sb("lnB_g", [128, D], F32)
        lnB_b = sb("lnB_b", [128, D], F32)
        ident_f = sb("ident_f", [128, 128], F32)
        ident_b = sb("ident_b", [128, 128], BF16)
        lstrict = sb("lstrict", [128, 128], BF16)
        ones_b = sb("ones_b", [128, 128], BF16)
        ones_f = sb("ones_f", [1, 128], F32)
        tmp_f = sb("tmp_f", [128, 128], F32)
        iota32 = sb("iota32", [128, 32], F32)
        iotacap = sb("iotacap", [128, 32], F32)
        iotadiv8 = sb("iotadiv8", [128, 32], F32)
        lg8 = sb("lg8", [128, 8], F32)
        mhalf = sb("mhalf", [128, 1], F32)
        trash = sb("trash", [128, 1], F32)
        carry = sb("carry", [128, 32], F32)
        dest_i = sb("dest_i", [128, NT[0], 2], I32)
        wts = sb("wts", [128, NT[0], 2], F32)
        bmw = sb("bmw_sb", [128, 4, 160], F32)
        small = sb("small", [128, 8, 20], F32)
        rt = sb("rt", [128, 2, 256], F32)
        rtu = sb("rtu", [128, 2, 16], U32)
        mbf = sb("mbf", [128, 2, 64], BF16)

        RBYTES = 104 * 1024
        R = sb("R", [128, RBYTES // 4], F32)
        carve_off = [0]

        def carve(shape, dt, reset=False):
            if reset:
                carve_off[0] = 0
            esz = 4 if dt in (F32, I32, U32) else 2
            n = int(np.prod(shape[1:]))
            nbytes = (n * esz + 31) // 32 * 32
            a = carve_off[0] // 4
            b = (carve_off[0] + nbytes) // 4
            assert carve_off[0] + nbytes <= RBYTES, ("region overflow", carve_off[0] + nbytes)
            carve_off[0] += nbytes
            v = R[:, a:b]
            if esz == 2:
                v = v.bitcast(dt)
            elif dt != F32:
                v = v.bitcast(dt)
            v = v[:, 0:n]
            if len(shape) == 3:
                v = v.rearrange("p (k n) -> p k n", k=shape[1])
            return v

        xn_t = [carve([128, D], F32, reset=(i == 0)) for i in range(4)]
        hx = carve([128, D], F32)
        xnTh = carve([128, 8, 128], BF16)
        xnT = [carve([128, 8, W], BF16) for _ in range(2)]
        zb = [carve([128, W], F32) for _ in range(6)]
        mt = [carve([128, W], F32) for _ in range(6)]
        catT2 = [carve([128, 8, CH], BF16) for _ in range(2)]
        pooledT = [carve([128, CH], BF16) for _ in range(2)]
        r_t = [carve([128, D], F32) for _ in range(1)]
        x1_t = [carve([128, D], F32) for _ in range(2)]
        x1bf = [carve([128, D], BF16) for _ in range(2)]
        x1T32 = carve([128, 8, 128], F32)
        x1Tb = [carve([128, 8, 128], BF16) for _ in range(2)]
        x1Tlo = carve([128, 8, 128], BF16)
        gate_t = [carve([128, D], F32) for _ in range(2)]
        p_t = [carve([128, DPLE], F32) for _ in range(4)]
        xbf_t = [carve([128, D], BF16) for _ in range(2)]
        pT = [carve([128, 2, 128], BF16) for _ in range(2)]
        a_end = carve_off[0]
        w1_sb = [carve([128, 8, DE], BF16, reset=(i == 0)) for i in range(2)]
        w3_sb = [carve([128, 8, DE], BF16) for _ in range(2)]
        w2_sb = [carve([128, 4, D], BF16) for _ in range(2)]
        xs_sb = [carve([128, 3, D], BF16) for _ in range(2)]
        xsT = carve([128, 8, CAP], BF16)
        hT = carve([128, 4, CAP], BF16)
        silu_t = [carve([128, CAP], F32) for _ in range(2)]
        ys_sb = carve([128, 3, D], F32)
        NCB = 4
        cy0 = [carve([128, D], F32, reset=(i == 0)) for i in range(NCB)]
        cy1 = [carve([128, D], F32) for _ in range(NCB)]
        cr = [carve([128, D], F32) for _ in range(NCB)]

        P01 = es.enter_context(nc.psum_tensor("P01", [128, 1024], F32))
        P23 = es.enter_context(nc.psum_tensor("P23", [128, 1024], F32))
        P45 = es.enter_context(nc.psum_tensor("P45", [128, 1024], F32))
        P67 = es.enter_context(nc.psum_tensor("P67", [128, 1024], F32))
        PT_bf = P01[:, 0:512].bitcast(BF16)

        REG = "REGION"

        def dma(q, out, in_, reads=(), writes=()):
            return S.add(q, lambda e: e.dma_start(out=out, in_=in_), reads, writes, dma=True)

        def tt(eng, out, a, b, op, reads, writes):
            return S.add(eng, lambda e: e.tensor_tensor(out=out, in0=a, in1=b, op=op), reads, writes)

        def ts(eng, out, a, s1, s2, op0, op1, reads, writes):
            if s2 is None:
                return S.add(eng, lambda e: e.tensor_scalar(out=out, in0=a, scalar1=s1, scalar2=None, op0=op0), reads, writes)
            return S.add(eng, lambda e: e.tensor_scalar(out=out, in0=a, scalar1=s1, scalar2=s2, op0=op0, op1=op1), reads, writes)

        def stt(eng, out, a, s, b, op0, op1, reads, writes):
            return S.add(eng, lambda e: e.scalar_tensor_tensor(out=out, in0=a, scalar=s, in1=b, op0=op0, op1=op1), reads, writes)

        def actf(out, in_, func, reads, writes, bias=None, scale=None, accum=None):
            kw = {}
            if bias is not None:
                kw["bias"] = bias
            if scale is not None:
                kw["scale"] = scale
            if accum is not None:
                kw["accum_out"] = accum
            return S.add("act", lambda e: e.activation(out=out, in_=in_, func=func, **kw), reads, writes)

        def cp(eng, out, in_, reads, writes):
            if eng == "act":
                return S.add(eng, lambda e: e.activation(out=out, in_=in_, func=AF.Copy), reads, writes)
            return S.add(eng, lambda e: e.tensor_copy(out=out, in_=in_), reads, writes)

        def mm(out, lhsT, rhs, start, stop, reads, writes):
            return S.add("pe", lambda e: e.matmul(out, lhsT=lhsT, rhs=rhs, start=start, stop=stop), reads, writes)

        def tr(out, in_, ident, reads, writes):
            return S.add("pe", lambda e: e.transpose(out=out, in_=in_, identity=ident), reads, writes)

        def scatter(dst, idx, src, reads, writes):
            return S.add("pool", lambda e: e.indirect_dma_start(
                out=dst, out_offset=bass.IndirectOffsetOnAxis(ap=idx, axis=0), in_=src, in_offset=None),
                reads, writes, dma=True)

        def gather(dst, src, idx, reads, writes):
            return S.add("pool", lambda e: e.indirect_dma_start(
                out=dst, out_offset=None, in_=src, in_offset=bass.IndirectOffsetOnAxis(ap=idx, axis=0)),
                reads, writes, dma=True)

        small_ctr = [0]

        def layernorm(src, src_res, dst, dst_res, g_t, b_t, gb_res, beta_eng="pool"):
            k = small_ctr[0] % 8
            small_ctr[0] += 1
            sm = small[:, k, :]
            sres = ("small", k)
            st = sm[:, 0:12]
            mv = sm[:, 12:14]
            std = sm[:, 14:15]
            rstd = sm[:, 15:16]
            nmr = sm[:, 16:17]
            S.add("dve", lambda e: e.bn_stats(out=st[:, 0:6], in_=src[:, 0:512]), [src_res], [sres])
            S.add("dve", lambda e: e.bn_stats(out=st[:, 6:12], in_=src[:, 512:1024]), [src_res, sres], [sres])
            S.add("dve", lambda e: e.bn_aggr(out=mv, in_=st), [sres], [sres])
            ts("dve", std, mv[:, 1:2], EPS, None, ALU.add, None, [sres], [sres])
            tt("pool", rstd, std, mhalf[:, 0:1], ALU.pow, [sres, "mhalf"], [sres])
            stt("dve", nmr, mv[:, 0:1], -1.0, rstd, ALU.mult, ALU.mult, [sres], [sres])
            ts("dve", dst, src, rstd, nmr, ALU.mult, ALU.add, [src_res, sres], [dst_res])
            tt("dve", dst, dst, g_t, ALU.mult, [dst_res, gb_res], [dst_res])
            tt(beta_eng, dst, dst, b_t, ALU.add, [dst_res, gb_res], [dst_res])

        S.add("pool", lambda e: e.memset(tmp_f[:], 0.0), [], ["tmp_f"])
        S.add("pool", lambda e: e.affine_select(out=ident_f[:], in_=tmp_f[:], pattern=[[-1, 128]], compare_op=ALU.not_equal,
                                                fill=1.0, base=0, channel_multiplier=1), ["tmp_f"], ["ident_f"])
        cp("dve", ident_b[:], ident_f[:], ["ident_f"], ["ident_b"])
        S.add("pool", lambda e: e.memset(tmp_f[:], 1.0), ["tmp_f"], ["tmp_f"])
        cp("dve", ones_b[:], tmp_f[:], ["tmp_f"], ["ones_b"])
        cp("dve", ones_f[:], tmp_f[0:1, :], ["tmp_f"], ["ones_f"])
        S.add("pool", lambda e: e.iota(tmp_f[:], pattern=[[1, 128]], base=0, channel_multiplier=-1,
                                       allow_small_or_imprecise_dtypes=True), ["tmp_f"], ["tmp_f"])
        ts("dve", lstrict[:], tmp_f[:], 0.0, None, ALU.is_gt, None, ["tmp_f"], ["lstrict"])
        S.add("pool", lambda e: e.iota(iota32[:], pattern=[[1, 32]], base=0, channel_multiplier=0,
                                       allow_small_or_imprecise_dtypes=True), [], ["iota32"])
        ts("dve", iotacap[:], iota32[:], float(CAP), None, ALU.mult, None, ["iota32"], ["iotacap"])
        S.add("pool", lambda e: e.iota(iotadiv8[:].rearrange("p (a b) -> p a b", a=4), pattern=[[1, 4], [0, 8]], base=0, channel_multiplier=0,
                                       allow_small_or_imprecise_dtypes=True), [], ["iotadiv8"])
        S.add("pool", lambda e: e.memset(lg8[:], -1e30), [], ["lg8"])
        S.add("pool", lambda e: e.memset(mhalf[:], -0.5), [], ["mhalf"])
        S.add("pool", lambda e: e.iota(trash[:], pattern=[[0, 1]], base=NE * CAP, channel_multiplier=1,
                                       allow_small_or_imprecise_dtypes=True), [], ["trash"])
        S.add("pool", lambda e: e.memset(bg_sb[:], 0.0), [], ["bg"])
        S.add("pool", lambda e: e.memset(mbf[:], 0.0), [], ["mbfz", ("mbf", 0), ("mbf", 1)])
        S.add("pool", lambda e: e.memset(hx, 0.0), [REG], ["hx"] + [("hxq", q_, s_) for q_ in range(8) for s_ in range(2)])
        dma("sp", bmw[:].rearrange("p a c -> p (a c)"), bmw_d.partition_broadcast(128), [], ["bmw"])
        dma("sp", tokinfo[:], tokinfo_d, [], ["tokinfo"])

        def load_layer_weights(l):
            for q4 in range(4):
                dma("pool", w_in_sb[:, :, q4 * 512:(q4 + 1) * 512],
                    w_in_d[l, :, q4 * 512:(q4 + 1) * 512].rearrange("(k p) f -> p k f", p=128), [], [("w_in", q4)])
            for q2 in range(2):
                dma("pool", w_o_sb[:, :, q2 * 512:(q2 + 1) * 512],
                    w_o_d[l, :, q2 * 512:(q2 + 1) * 512].rearrange("(k p) f -> p k f", p=128), [], [("w_o", q2)])
                dma("pool", wg_sb[:, :, q2 * 512:(q2 + 1) * 512],
                    wg_d[l, :, q2 * 512:(q2 + 1) * 512].rearrange("(k p) f -> p k f", p=128), [], [("wg", q2)])
            dma("pool", wp_sb[:], wp_d[l].rearrange("(k p) f -> p k f", p=128), [], ["wp"])
            ts("dve", wp_sb[:].rearrange("p k n -> p (k n)"), wp_sb[:].rearrange("p k n -> p (k n)"), 0.5, None, ALU.mult, None, ["wp"], ["wp"])
            dma("pool", poolw_sb[:], pool_w_d[l].rearrange("g c d -> c g d"), [], ["poolw"])
            dma("pool", bg_sb[0:1, :], bg_d[l], [], ["bg"])
            dma("sp", wr_sb[:], wr_d[l].rearrange("(k p) f -> p k f", p=128), [], ["wr"])
            cp("dve", wr_hi[:], wr_sb[:], ["wr"], ["wrh"])
            tt("dve", wr_lo[:], wr_sb[:], wr_hi[:], ALU.subtract, ["wr", "wrh"], ["wrl"])
            dma("sp", br_sb[:], br_d[l].partition_broadcast(128), [], ["br"])
            dma("sp", cols[:], cols_d[l], [], ["cols"])
            dma("sp", lnA_g[:], lnv_d[l, 0:1, :].partition_broadcast(128), [], ["lnA"])
            dma("sp", lnA_b[:], lnv_d[l, 1:2, :].partition_broadcast(128), [], ["lnA"])

        def load_lnB(l, which):
            if which == 0:
                dma("sp", lnB_g[:], ln0_d[0:1, :].partition_broadcast(128), [], ["lnB"])
                dma("sp", lnB_b[:], ln0_d[1:2, :].partition_broadcast(128), [], ["lnB"])
            else:
                dma("sp", lnB_g[:], lnv_d[l, 2:3, :].partition_broadcast(128), [], ["lnB"])
                dma("sp", lnB_b[:], lnv_d[l, 3:4, :].partition_broadcast(128), [], ["lnB"])

        def phase_a(l):
            nch = KNCH if KNCH else NCHUNK[l]
            base_i = 8 if l == 0 else 136
            src_x = xpad if l == 0 else xcur
            ti_base = 0
            S.add("pool", lambda e: e.memset(carry[:], 0.0), [], ["carry"])

            def xres_reads(i0, n):
                if l == 0:
                    return []
                t0 = (i0 - 8) // 128
                t1 = (i0 + n - 1 - 8) // 128
                return [("xc", t) for t in range(t0, t1 + 1)]

            def load_halo_group(G):
                for q in range(8):
                    j = 8 * G + q
                    if j >= nch:
                        break
                    i0 = base_i + CH * j
                    dma("sp", hx[16 * q:16 * q + 8, :], src_x[i0 - 8:i0, :], [REG, "hxdone"] + xres_reads(i0 - 8, 8), [("hxq", q, 0)])
                    dma("sp", hx[16 * q + 8:16 * q + 16, :], src_x[i0 + CH:i0 + CH + 8, :], [REG, "hxdone"] + xres_reads(i0 + CH, 8), [("hxq", q, 1)])
                hxall = [("hxq", q_, s_) for q_ in range(8) for s_ in range(2)]
                S.add("pool", lambda e: e.tensor_copy(out=hx[0:1, 0:1], in_=hx[0:1, 0:1]), hxall + ["hx"], hxall + ["hx"])
                if l == 0:
                    layernorm(hx, "hx", hx, "hx", lnB_g[:], lnB_b[:], "lnB")
                hb = xbf_t[0]
                cp("dve", hb, hx, ["hx", REG], [("xbf", 0), "hxdone"])
                for k in range(8):
                    tr(PT_bf[:, k * 128:(k + 1) * 128], hb[:, k * 128:(k + 1) * 128], ident_b[:], [("xbf", 0), "ident_b"], ["PT"])
                cp("act", xnTh, PT_bf.rearrange("p (k n) -> p k n", k=8), ["PT"], ["xnTh"])

            def load_chunk_x(j):
                if j >= nch:
                    return
                i0 = base_i + CH * j
                for tt_ in range(2):
                    slot = (2 * j + tt_) % 4
                    dma("sp", xn_t[slot], src_x[i0 + 128 * tt_:i0 + 128 * tt_ + 128, :],
                        [REG] + xres_reads(i0 + 128 * tt_, 128), [("xn", slot)])

            def load_chunk_p(j):
                if j >= nch:
                    return
                for tt_ in range(2):
                    tix = 2 * j + tt_
                    prow = tix * 128 if l == 0 else 128 + tix * 128
                    dma("sp", p_t[tix % 4], ppad[l, prow:prow + 128, :], [REG], [("p", tix % 4)])

            def front(j):
                catT = catT2[j % 2]
                cres = ("catT", j % 2)
                PA, PAr = P67[:, 0:W], "P6"
                PB, PBr = P67[:, 512:512 + W], "P7"
                xb = xnT[j % 2]
                xres = ("xnT", j % 2)
                q = j % 8
                for tt_ in range(2):
                    slot = (2 * j + tt_) % 4
                    if l == 0:
                        layernorm(xn_t[slot], ("xn", slot), xn_t[slot], ("xn", slot), lnB_g[:], lnB_b[:], "lnB", beta_eng="dve")
                    xbf = xbf_t[tt_]
                    cp("act", xbf, xn_t[slot], [("xn", slot), REG], [("xbf", tt_)])
                    for k in range(8):
                        tr(PT_bf[:, k * 128:(k + 1) * 128], xbf[:, k * 128:(k + 1) * 128], ident_b[:],
                           [("xbf", tt_), "ident_b"], ["PT"])
                    cp(os.environ.get("KXNT", "dve"), xb[:, :, 8 + 128 * tt_:8 + 128 * tt_ + 128], PT_bf.rearrange("p (k n) -> p k n", k=8),
                       ["PT"], [xres])
                    yield
                cp("pool", xb[:, :, 0:8], xnTh[:, :, 16 * q:16 * q + 8], ["xnTh"], [xres])
                cp("pool", xb[:, :, CH + 8:CH + 16], xnTh[:, :, 16 * q + 8:16 * q + 16], ["xnTh"], [xres])

                bidx = None
                for bi, (bl, bj, bws) in enumerate(BWIN):
                    if bl == l and bj == j:
                        bidx, ws = bi, bws

                def zmat(fc, pout, pres):
                    for k in range(8):
                        mm(pout, w_in_sb[:, k, fc * 128:(fc + 1) * 128], xb[:, k, :], k == 0, k == 7,
                           [("w_in", fc // 4), xres], [pres])

                for i in range(4):
                    s3 = (i % 2) * 3
                    zh, zgb, zgc = zb[s3], zb[s3 + 1], zb[s3 + 2]
                    rh, rgb, rgc = ("zb", s3), ("zb", s3 + 1), ("zb", s3 + 2)
                    zmat(i, PA, PAr)
                    actf(zh, PA, AF.Identity, [PAr, "cols", REG], [rh], bias=cols[:, i:i + 1])
                    yield
                    zmat(8 + i, PB, PBr)
                    actf(zgc, PB, AF.Identity, [PBr, "cols", REG], [rgc], bias=cols[:, 8 + i:9 + i])
                    yield
                    zmat(4 + i, PA, PAr)
                    actf(zgb, PA, AF.Identity, [PAr, "cols", REG], [rgb], bias=cols[:, 4 + i:5 + i])
                    yield
                    v = mt[0]
                    t1 = mt[1]
                    ce = "pool" if (i % 2 == 1 and os.environ.get("KCONVPOOL", "0") == "1") else "dve"
                    tt(ce, v, zgc, zh, ALU.mult, [rh, rgc, REG], [("mt", 0)])
                    if bidx is not None:
                        tt("dve", v[:, ws:ws + 32], v[:, ws:ws + 32], bmw[:, bidx, 0:32], ALU.mult, [("mt", 0), "bmw"], [("mt", 0)])
                    c0, c1, c2 = 16 + 0 * 4 + i, 16 + 1 * 4 + i, 16 + 2 * 4 + i
                    ts(ce, t1[:, 0:CH], v[:, 7:7 + CH], cols[:, c0:c0 + 1], None, ALU.mult, None, [("mt", 0), "cols"], [("mt", 1)])
                    stt(ce, t1[:, 0:CH], v[:, 8:8 + CH], cols[:, c1:c1 + 1], t1[:, 0:CH], ALU.mult, ALU.add, [("mt", 0), ("mt", 1), "cols"], [("mt", 1)])
                    stt(ce, t1[:, 0:CH], v[:, 9:9 + CH], cols[:, c2:c2 + 1], t1[:, 0:CH], ALU.mult, ALU.add, [("mt", 0), ("mt", 1), "cols"], [("mt", 1)])
                    tt(ce, catT[:, i, :], zgb[:, 8:8 + CH], t1[:, 0:CH], ALU.mult, [rgb, ("mt", 1), REG], [cres])
                    yield
                for g in range(4):
                    wdt = POOL_W[g]
                    zu = zb[g % 2 * 3]
                    ru = ("zb", g % 2 * 3)
                    pz, pzr = (PA, PAr) if g % 2 == 0 else (PB, PBr)
                    py, pyr = (PB[:, 0:CH], PBr) if g % 2 == 0 else (PA[:, 0:CH], PAr)
                    zmat(12 + g, pz, pzr)
                    actf(zu, pz, AF.Identity, [pzr, "cols", REG], [ru], bias=cols[:, 12 + g:13 + g])
                    yield
                    if bidx is not None:
                        tt("dve", zu[:, ws:ws + 32], zu[:, ws:ws + 32], bmw[:, bidx, 0:32], ALU.mult, [ru, "bmw"], [ru])
                    a1, a2, a3 = mt[2], mt[3], mt[4]
                    ssum = mt[5]
                    if wdt == 2:
                        tt("dve", ssum[:, 8:8 + CH], zu[:, 7:7 + CH], zu[:, 8:8 + CH], ALU.add, [ru, REG], [("mt", 5)])
                    else:
                        tt("dve", a1[:, 0:W - 1], zu[:, 0:W - 1], zu[:, 1:W], ALU.add, [ru, REG], [("mt", 2)])
                        if wdt == 4:
                            tt("dve", ssum[:, 8:8 + CH], a1[:, 6:6 + CH], a1[:, 8:8 + CH], ALU.add, [("mt", 2)], [("mt", 5)])
                        else:
                            tt("dve", a2[:, 0:W - 3], a1[:, 0:W - 3], a1[:, 2:W - 1], ALU.add, [("mt", 2)], [("mt", 3)])
                            if wdt == 8:
                                tt("dve", ssum[:, 8:8 + CH], a2[:, 4:4 + CH], a2[:, 8:8 + CH], ALU.add, [("mt", 3)], [("mt", 5)])
                            else:
                                tt("dve", a3[:, 0:W - 7], a2[:, 0:W - 7], a2[:, 4:W - 3], ALU.add, [("mt", 3)], [("mt", 4)])
                                tt("dve", ssum[:, 8:8 + CH], a3[:, 0:CH], a3[:, 8:8 + CH], ALU.add, [("mt", 4)], [("mt", 5)])
                    pl = pooledT[g % 2]
                    plr = ("pooledT", g % 2)
                    stt("dve", pl, ssum[:, 8:8 + CH], 1.0 / wdt, zu[:, 8:8 + CH], ALU.mult, ALU.subtract, [("mt", 5), ru, REG], [plr])
                    if bidx is not None:
                        lo = max(ws, 8)
                        hi = min(ws + 32, 8 + CH)
                        tmpw = mt[2]
                        tt("dve", tmpw[:, lo:hi], ssum[:, lo:hi], bmw[:, bidx, 32 * (1 + g) + (lo - ws):32 * (1 + g) + (hi - ws)],
                           ALU.mult, [("mt", 5), "bmw", ("mt", 2)], [("mt", 2)])
                        tt("dve", pl[:, lo - 8:hi - 8], tmpw[:, lo:hi], zu[:, lo:hi], ALU.subtract, [("mt", 2), ru, plr], [plr])
                    mm(py, poolw_sb[:, g, :], pl, True, True, ["poolw", plr], [pyr])
                    actf(catT[:, 4 + g, :], py, AF.Identity, [pyr, "cols", REG], [cres], scale=cols[:, 28 + g:29 + g])
                    yield

            def back(j, tiles=(0, 1)):
                catT = catT2[j % 2]
                cres = ("catT", j % 2)
                for tt_ in tiles:
                    tix = 2 * j + tt_
                    slot = (2 * j + tt_) % 4
                    b2 = tix % 2
                    for half in range(2):
                        for k in range(8):
                            mm(P45[:, half * 512:(half + 1) * 512], catT[:, k, tt_ * 128:(tt_ + 1) * 128],
                               w_o_sb[:, k, half * 512:(half + 1) * 512], k == 0, k == 7, [cres, ("w_o", half)], ["P45"])
                    MARKS.setdefault("mix_done", len(S.ops))
                    r = r_t[0]
                    stt("dve", r, xn_t[slot], ALPHA, P45[:, :], ALU.mult, ALU.add, [("xn", slot), "P45", REG], [("r", 0)])
                    if tt_ == 1:
                        load_chunk_x(j + 2)
                    yield
                    x1 = x1_t[b2]
                    layernorm(r, ("r", 0), x1, ("x1", b2), lnA_g[:], lnA_b[:], "lnA", beta_eng="dve")
                    yield
                    MARKS.setdefault("ln1_done", len(S.ops))
                    cp("act", x1bf[b2], x1, [("x1", b2)], [("x1bf", b2)])
                    for k in range(8):
                        tr(P23[:, k * 128:(k + 1) * 128], x1[:, k * 128:(k + 1) * 128], ident_f[:], [("x1", b2), "ident_f"], ["P2", "P3"])
                    yield
                    cp("dve", x1T32, P23.rearrange("p (k n) -> p k n", k=8), ["P2", "P3", REG], ["x1T32"])
                    if os.environ.get("KX1TB", "dve") == "act":
                        for hb_ in range(2):
                            cp("act", x1Tb[b2][:, 4 * hb_:4 * hb_ + 4, :], P23[:, 512 * hb_:512 * hb_ + 512].rearrange("p (k n) -> p k n", k=4),
                               ["P2", "P3", REG], [("x1Tb", b2)])
                    else:
                        cp("dve", x1Tb[b2], P23.rearrange("p (k n) -> p k n", k=8), ["P2", "P3", REG], [("x1Tb", b2)])
                    MARKS.setdefault("x1T_done", len(S.ops))
                    yield
                    PL = P01[:, 512:576]
                    if os.environ.get("KROUTER", "f32") == "f32":
                        for k in range(8):
                            mm(PL, x1T32[:, k, :], wr_sb[:, k, :], k == 0, k == 7, ["x1T32", "wr"], ["P1"])
                    else:
                        tt("dve", x1Tlo, x1T32, x1Tb[b2], ALU.subtract, ["x1T32", ("x1Tb", b2)], ["x1Tlo"])
                        for k in range(8):
                            mm(PL, x1Tb[b2][:, k, :], wr_hi[:, k, :], k == 0, False, [("x1Tb", b2), "wrh"], ["P1"])
                            mm(PL, x1Tb[b2][:, k, :], wr_lo[:, k, :], False, False, [("x1Tb", b2), "wrl"], ["P1"])
                            mm(PL, x1Tlo[:, k, :], wr_hi[:, k, :], False, k == 7, ["x1Tlo", "wrh"], ["P1"])
                    MARKS.setdefault("logits_done", len(S.ops))
                    yield
                    yield from route(l, tix, PL)
                    MARKS.setdefault("route_done", len(S.ops))
                    for kk in range(2):
                        if STAGE != "a0":
                            scatter(xs_d[l], dest_i[:, tix, kk:kk + 1], x1bf[b2], [("x1bf", b2), ("dest", tix)], [("xsw", l, tix, kk)])
                    yield
                    for half in range(2):
                        PG = P45[:, half * 512:(half + 1) * 512]
                        for k in range(8):
                            mm(PG, x1Tb[b2][:, k, :], wg_sb[:, k, half * 512:(half + 1) * 512], k == 0, False, [("x1Tb", b2), ("wg", half)], ["P45"])
                        mm(PG, ones_b[:], bg_sb[:, half * 512:(half + 1) * 512], False, True, ["ones_b", "bg"], ["P45"])
                    gt = gate_t[b2]
                    for half in range(2):
                        actf(gt[:, half * 512:(half + 1) * 512], P45[:, half * 512:(half + 1) * 512], AF.Tanh, ["P45", REG], [("gate", b2)], scale=0.5)
                    MARKS.setdefault("gate_done", len(S.ops))
                    yield
                    ptile = p_t[tix % 4]
                    PP = P01[:, 704:960]
                    for k in range(2):
                        tr(PP[:, k * 128:(k + 1) * 128], ptile[:, k * 128:(k + 1) * 128], ident_f[:], [("p", tix % 4), "ident_f"], ["P1"])
                    cp("dve", pT[b2], PP.rearrange("p (k n) -> p k n", k=2), ["P1", REG], [("pT", b2)])
                    for half in range(2):
                        for k in range(2):
                            mm(P23[:, half * 512:(half + 1) * 512], pT[b2][:, k, :], wp_sb[:, k, half * 512:(half + 1) * 512],
                               k == 0, k == 1, [("pT", b2), "wp"], ["P2", "P3"])
                    yield
                    stt("dve", gt, gt, 1.0, P23[:, :], ALU.add, ALU.mult, [("gate", b2), "P2", "P3"], [("gate", b2)])
                    stt("dve", gt, x1, ALPHA, gt, ALU.mult, ALU.add, [("x1", b2), ("gate", b2)], [("gate", b2)])
                    dma("sp", r2p_d[l][tix * 128:(tix + 1) * 128, :], gt, [("gate", b2)], [("r2p", l, tix)])
                    yield
                if 1 in tiles:
                    load_chunk_p(j + 2)

            def run_gens(gens):
                gens = list(gens)
                while gens:
                    for g_ in list(gens):
                        try:
                            next(g_)
                        except StopIteration:
                            gens.remove(g_)

            load_chunk_x(0)
            load_chunk_p(0)
            load_chunk_x(1)
            load_chunk_p(1)
            load_halo_group(0)
            run_gens([front(0)])
            for j in range(nch):
                if os.environ.get("KSPLIT", "0") == "1":
                    gens = [back(j, (0,)), back(j, (1,))]
                else:
                    gens = [back(j)]
                if j + 1 < nch:
                    if (j + 1) % 8 == 0:
                        load_halo_group((j + 1) // 8)
                    if os.environ.get("KORDER", "back") == "front":
                        gens.insert(0, front(j + 1))
                    else:
                        gens.append(front(j + 1))
                run_gens(gens)

        def route(l, tix, PL):
            k = tix % 2
            rs = ("rt", k)
            A = rt[:, k, :]
            U = rtu[:, k, :]
            lgt = A[:, 0:36]
            mx8 = A[:, 40:48]
            negmax = A[:, 48:49]
            ex4 = A[:, 52:56]
            sume = A[:, 56:57]
            gw = A[:, 57:58]
            gself = A[:, 58:59]
            gsel8 = A[:, 59:60]
            oh4 = A[:, 60:64]
            el = A[:, 64:72]
            tv8 = A[:, 72:80]
            dd = A[:, 80:81]
            ee = A[:, 81:82]
            den = A[:, 82:83]
            w0 = A[:, 83:84]
            w1 = A[:, 84:85]
            tif = A[:, 86:88]
            eid = A[:, 88:90]
            oh0 = A[:, 96:128]
            oh1 = A[:, 128:160]
            rank = A[:, 160:192]
            tmp32 = A[:, 192:224]
            destf = A[:, 224:226]
            lg8k = A[:, 232:240]
            gi8 = U[:, 0:8]
            ti8 = U[:, 8:16]
            tt("dve", lgt, PL[:, 0:36], br_sb[:, 0:36], ALU.add, ["P1", "br"], [rs])
            cp("dve", lg8k, lg8[:], [rs, "lg8"], [rs])
            cp("dve", lg8k[:, 0:4], lgt[:, 0:4], [rs], [rs])
            S.add("dve", lambda e: e.max(out=mx8, in_=lg8k), [rs], [rs])
            S.add("dve", lambda e: e.max_index(out=gi8, in_max=mx8, in_values=lg8k), [rs], [rs])
            ts("dve", negmax, mx8[:, 0:1], -1.0, None, ALU.mult, None, [rs], [rs])
            actf(ex4, lgt[:, 0:4], AF.Exp, [rs], [rs], bias=negmax, scale=1.0, accum=sume)
            yield
            cp("dve", gself, gi8[:, 0:1], [rs], [rs])
            pen = oh0
            elm = oh1
            ts("dve", pen, iotadiv8[:], gself, -1e30, ALU.not_equal, ALU.mult, [rs, "iotadiv8"], [rs])
            tt("dve", elm, lgt[:, 4:36], pen, ALU.add, [rs], [rs])
            S.add("dve", lambda e: e.max(out=tv8, in_=elm), [rs], [rs])
            S.add("dve", lambda e: e.max_index(out=ti8, in_max=tv8, in_values=elm), [rs], [rs])
            yield
            tt("dve", dd, tv8[:, 1:2], tv8[:, 0:1], ALU.subtract, [rs], [rs])
            actf(ee, dd, AF.Exp, [rs], [rs])
            cp("dve", eid, ti8[:, 0:2], [rs], [rs])
            tcol = (l * NT[0] + tix) * 2
            yield
            maybe_invalid = (l == 0 and tix in (0, NT[0] - 1))
            if maybe_invalid:
                ts("dve", oh0, iota32[:], eid[:, 0:1], tokinfo[:, tcol:tcol + 1], ALU.is_equal, ALU.mult, [rs, "iota32", "tokinfo"], [rs])
                ts("dve", oh1, iota32[:], eid[:, 1:2], tokinfo[:, tcol:tcol + 1], ALU.is_equal, ALU.mult, [rs, "iota32", "tokinfo"], [rs])
            else:
                ts("dve", oh0, iota32[:], eid[:, 0:1], None, ALU.is_equal, None, [rs, "iota32"], [rs])
                ts("dve", oh1, iota32[:], eid[:, 1:2], None, ALU.is_equal, None, [rs, "iota32"], [rs])
            mres = ("mbf", k)
            tt("dve", mbf[:, k, 0:32], oh0, oh1, ALU.add, [rs, "mbfz"], [mres])
            S.add("dve", lambda e: e.reciprocal(out=gw, in_=sume), [rs], [rs])
            ts("dve", den, ee, 1.0, None, ALU.add, None, [rs], [rs])
            S.add("dve", lambda e: e.reciprocal(out=w0, in_=den), [rs], [rs])
            tt("dve", w1, ee, w0, ALU.mult, [rs], [rs])
            ts("dve", wts[:, tix, :], A[:, 83:85], gw, None, ALU.mult, None, [rs], [("wts", tix)])
            PR = P01[:, 576:704]
            mm(PR[:, 0:64], lstrict[:], mbf[:, k, :], True, True, ["lstrict", mres], ["P1"])
            mm(PR[:, 64:128], ones_b[:], mbf[:, k, :], True, True, ["ones_b", mres], ["P1"])
            yield
            tt("dve", rank, PR[:, 0:32], carry[:], ALU.add, ["P1", "carry", rs], [rs])
            tt("dve", carry[:], PR[:, 64:96], carry[:], ALU.add, ["P1", "carry"], ["carry"])
            rsel = A[:, 240:242]
            ovf = A[:, 242:244]
            keep = A[:, 244:246]
            tt("dve", tmp32, rank, oh0, ALU.mult, [rs], [rs])
            S.add("dve", lambda e: e.reduce_sum(out=rsel[:, 0:1], in_=tmp32, axis=AX.X), [rs], [rs])
            tt("dve", tmp32, rank, oh1, ALU.mult, [rs], [rs])
            S.add("dve", lambda e: e.reduce_sum(out=rsel[:, 1:2], in_=tmp32, axis=AX.X), [rs], [rs])
            yield
            stt("dve", destf, eid, float(CAP), rsel, ALU.mult, ALU.add, [rs], [rs])
            ts("dve", ovf, rsel, float(CAP) - 0.5, None, ALU.is_gt, None, [rs], [rs])
            ts("dve", keep, ovf, -1.0, 1.0, ALU.mult, ALU.add, [rs], [rs])
            if maybe_invalid:
                ts("dve", destf, destf, tokinfo[:, tcol:tcol + 1], None, ALU.mult, None, [rs, "tokinfo"], [rs])
            tt("dve", destf, destf, keep, ALU.mult, [rs], [rs])
            stt("dve", destf, ovf, trash[:, 0:1], destf, ALU.mult, ALU.add, [rs, "trash"], [rs])
            if maybe_invalid:
                ts("dve", destf, destf, tokinfo[:, tcol + 1:tcol + 2], None, ALU.add, None, [rs, "tokinfo"], [rs])
            tt("dve", wts[:, tix, :], wts[:, tix, :], keep, ALU.mult, [rs, ("wts", tix)], [("wts", tix)])
            cp("dve", dest_i[:, tix, :], destf, [rs], [("dest", tix)])
            if DEBUG:
                cp("dve", A[:, 226:228], destf, [rs], [rs])
                cp("dve", A[:, 228:230], wts[:, tix, :], [rs, ("wts", tix)], [rs])
                dma("sp", dbg_route[l, :, tix * 4:tix * 4 + 4], A[:, 226:230], [rs], [("dbgr", l, tix)])

        def phase_b(l):
            nt = NT[l]
            xs_reads = [("xsw", l, t, kk) for t in range(nt) for kk in range(2)]

            def load_e(e):
                b = e % 2
                first = [REG] if e < 2 else []
                wres = ("ew", b)
                for hh in range(2):
                    dma("pool", w1_sb[b][:, 4 * hh:4 * hh + 4, :],
                        w1_d[l, e].rearrange("(p k) f -> p k f", k=8)[:, 4 * hh:4 * hh + 4, :], [], [("ew", b, 1, hh)] + (first if hh == 0 else []))
                    dma("pool", w3_sb[b][:, 4 * hh:4 * hh + 4, :],
                        w3_d[l, e].rearrange("(p k) f -> p k f", k=8)[:, 4 * hh:4 * hh + 4, :], [], [("ew", b, 3, hh)])
                    dma("pool", w2_sb[b][:, 2 * hh:2 * hh + 2, :],
                        w2_d[l, e].rearrange("(k p) f -> p k f", p=128)[:, 2 * hh:2 * hh + 2, :], [], [("ew", b, 2, hh)])
                dma("sp", xs_sb[b], xs_d[l][e * CAP:(e + 1) * CAP, :].rearrange("(j p) d -> p j d", p=128),
                    xs_reads, [("xs", b)] + first)

            def comp_e(e):
                b = e % 2
                wres = ("ew", b)
                for j in range(3):
                    for k in range(8):
                        tr(PT_bf[:, k * 128:(k + 1) * 128], xs_sb[b][:, j, :].rearrange("p (q k) -> p k q", k=8)[:, k, :], ident_b[:],
                           [("xs", b), "ident_b"], ["PT"])
                    eng = "act" if j % 2 == 0 else "dve"
                    cp(eng, xsT[:, :, j * 128:(j + 1) * 128], PT_bf.rearrange("p (k n) -> p k n", k=8), ["PT", REG], ["xsT"])
                for m in range(4):
                    p1 = P23[:, 0:CAP] if m % 2 == 0 else P23[:, 512:512 + CAP]
                    p1r = "P2" if m % 2 == 0 else "P3"
                    p3 = P45[:, 0:CAP] if m % 2 == 0 else P45[:, 512:512 + CAP]
                    p3r = "P4" if m % 2 == 0 else "P5"
                    for k in range(8):
                        mm(p1, w1_sb[b][:, k, m * 128:(m + 1) * 128], xsT[:, k, :], k == 0, k == 7, [("ew", b, 1, k // 4), "xsT"], [p1r])
                    for k in range(8):
                        mm(p3, w3_sb[b][:, k, m * 128:(m + 1) * 128], xsT[:, k, :], k == 0, k == 7, [("ew", b, 3, k // 4), "xsT"], [p3r])
                    sl = silu_t[m % 2]
                    actf(sl, p1, AF.Silu, [p1r, REG], [("silu", m % 2)])
                    tt("dve", hT[:, m, :], sl, p3, ALU.mult, [("silu", m % 2), p3r, REG], ["hT"])
                for j in range(3):
                    for half in range(2):
                        py = P67[:, half * 512:(half + 1) * 512]
                        pyr = "P6" if half == 0 else "P7"
                        for m in range(4):
                            mm(py, hT[:, m, j * 128:(j + 1) * 128], w2_sb[b][:, m, half * 512:(half + 1) * 512],
                               m == 0, m == 3, ["hT", ("ew", b, 2, m // 2)], [pyr])
                        eng = "act" if half == 0 else "dve"
                        cp(eng, ys_sb[:, j, half * 512:(half + 1) * 512], py, [pyr, REG], ["ys_sb"])
                dma("sp", ys_d[l][e * CAP:(e + 1) * CAP, :].rearrange("(j p) d -> p j d", p=128), ys_sb, ["ys_sb"], [("ysw", l, e)])

            load_e(0)
            for e in range(NE):
                if e + 1 < NE:
                    load_e(e + 1)
                comp_e(e)

        def phase_c(l):
            nt = NT[l]
            ys_reads = [("ysw", l, e) for e in range(NE)]
            stores = []

            def loads(t):
                b = t % NCB
                for kk, buf in ((0, cy0), (1, cy1)):
                    gather(buf[b], ys_d[l], dest_i[:, t, kk:kk + 1], ys_reads + [("dest", t), REG], [("cy", kk, b)])
                dma("sp", cr[b], r2p_d[l][t * 128:(t + 1) * 128, :], [("r2p", l, t), REG], [("cr", b)])

            def comp(t):
                b = t % NCB
                stt("dve", cr[b], cy0[b], wts[:, t, 0:1], cr[b], ALU.mult, ALU.add, [("cy", 0, b), ("cr", b), ("wts", t)], [("cr", b)])
                stt("dve", cr[b], cy1[b], wts[:, t, 1:2], cr[b], ALU.mult, ALU.add, [("cy", 1, b), ("cr", b), ("wts", t)], [("cr", b)])
                layernorm(cr[b], ("cr", b), cy0[b], ("cy", 0, b), lnB_g[:], lnB_b[:], "lnB", beta_eng="dve")
                if l == 0:
                    stores.append(dma("sp", xcur[8 + t * 128:8 + (t + 1) * 128, :], cy0[b], [("cy", 0, b)], [("xc", t)]))
                else:
                    stores.append(dma("sp", out_d[t * 128:(t + 1) * 128, :], cy0[b], [("cy", 0, b)], [("out", t)]))

            for b_ in range(NCB):
                S.add("pool", (lambda ap: (lambda e: e.memset(ap, 0.0)))(cy0[b_]), [], [("cy", 0, b_)])
                S.add("pool", (lambda ap: (lambda e: e.memset(ap, 0.0)))(cy1[b_]), [], [("cy", 1, b_)])
            dma("sp", ys_d[l][NE * CAP:NE * CAP + 128, :], cy0[0], [("cy", 0, 0)], [("ysw", l, "trash")])
            ys_reads.append(("ysw", l, "trash"))
            for t in range(min(NCB - 1, nt)):
                loads(t)
            for t in range(nt):
                if t + NCB - 1 < nt:
                    loads(t + NCB - 1)
                comp(t)
            return stores

        final = []
        load_layer_weights(0)
        load_lnB(0, 0)
        for l in range(DEPTH):
            if STAGE == "w":
                break
            phase_a(l)
            if STAGE in ("a0", "a0s"):
                break
            load_lnB(l, 1)
            if l + 1 < DEPTH:
                load_layer_weights(l + 1)
            S.barrier()
            phase_b(l)
            if STAGE == "b0":
                break
            S.barrier()
            final = phase_c(l)
            S.barrier()
            if STAGE == "c0":
                break
        dbg = [i for i, op in enumerate(S.ops) if op.dma] if DEBUG else []
        S.emit(nc, final_wait_ops=list(final) + dbg)
    return nc


def _boundary_consts(own_start):
    bm = np.zeros((4, 5, 32), np.float32)
    for bi, (l, j, ws) in enumerate(BWIN):
        for c in range(32):
            col = ws + c
            if l == 0:
                tok = own_start - 136 + CH * j + col
            else:
                tok = own_start + CH * j + col - 8
            valid = 0 <= tok < SEQ
            bm[bi, 0, c] = 1.0 if valid else 0.0
            for g, wdt in enumerate(POOL_W):
                left = wdt // 2
                right = wdt - 1 - left
                cnt = min(tok + right + 1, SEQ) - max(tok - left, 0)
                bm[bi, 1 + g, c] = (1.0 / cnt) if (valid and cnt > 0) else 0.0
    return bm.reshape(1, 640)


_NC_CACHE = {}


def kernel(x, p, ln0_g, ln0_b, w_in, b_in, conv_w, pool_w, pool_scale, w_o,
           ln1_g, ln1_b, w_router_group, b_router_group, w_router_expert,
           b_router_expert, w1, w3, w2, w_ple_gate, b_ple_gate, w_ple_proj,
           ln2_g, ln2_b):
    f32 = lambda a: np.ascontiguousarray(np.asarray(a), dtype=np.float32)
    x = f32(x); p = f32(p)
    L = DEPTH
    wr = np.concatenate([f32(w_router_group), f32(w_router_expert).transpose(0, 2, 1, 3).reshape(L, D, 32),
                         np.zeros((L, D, 28), np.float32)], axis=2)
    br = np.concatenate([f32(b_router_group), f32(b_router_expert).reshape(L, 32),
                         np.zeros((L, 28), np.float32)], axis=1).reshape(L, 1, 64)
    colsv = np.zeros((L, 128, 32), np.float32)
    colsv[:, :, 0:16] = f32(b_in).reshape(L, 16, 128).transpose(0, 2, 1)
    cw = f32(conv_w).reshape(L, 3, 4, 128)
    colsv[:, :, 16:28] = cw.transpose(0, 3, 1, 2).reshape(L, 128, 12)
    colsv[:, :, 28:32] = f32(pool_scale).reshape(L, 4, 128).transpose(0, 2, 1)
    ln0 = np.stack([f32(ln0_g), f32(ln0_b)], axis=0)
    lnv = np.stack([f32(ln1_g), f32(ln1_b), f32(ln2_g), f32(ln2_b)], axis=1)
    shared = {
        "ln0": ln0, "lnv": np.ascontiguousarray(lnv), "cols": colsv,
        "w_in": f32(w_in), "pool_w": f32(pool_w), "w_o": f32(w_o), "wr": np.ascontiguousarray(wr),
        "br": np.ascontiguousarray(br), "w1": f32(w1)[:, :NE_DECL], "w3": f32(w3)[:, :NE_DECL], "w2": f32(w2)[:, :NE_DECL],
        "w_ple_gate": f32(w_ple_gate), "b_ple_gate": f32(b_ple_gate).reshape(L, 1, D),
        "w_ple_proj": f32(w_ple_proj),
    }
    in_maps = []
    for c in range(NCORES):
        b, h = c // 2, c % 2
        own = h * OWN
        xp = np.zeros((XROWS, D), np.float32)
        t0 = own - 136
        lo, hi = max(t0, 0), min(t0 + XROWS, SEQ)
        xp[lo - t0:hi - t0] = x[b, lo:hi]
        pp = np.zeros((L, NT[0] * 128, DPLE), np.float32)
        t0p = own - 128
        lo, hi = max(t0p, 0), min(t0p + NT[0] * 128, SEQ)
        pp[:, lo - t0p:hi - t0p] = p[:, b, lo:hi]
        m = dict(shared)
        m["xpad"] = xp
        m["ppad"] = pp
        m["bmw"] = _boundary_consts(own)
        ti = np.zeros((128, DEPTH, NT[0], 2), np.float32)
        ti[:, 1, :, 0] = 1.0
        lidx = np.arange(NT[0])[None, :] * 128 + np.arange(128)[:, None]
        tok = own - 128 + lidx
        v0 = ((tok >= 0) & (tok < SEQ)).astype(np.float32)
        ti[:, 0, :, 0] = v0
        ti[:, 0, :, 1] = (1.0 - v0) * (float(NE * CAP) + np.arange(128, dtype=np.float32)[:, None])
        m["tokinfo"] = ti.reshape(128, DEPTH * NT[0] * 2)
        in_maps.append(m)
    if "nc" not in _NC_CACHE:
        _NC_CACHE["nc"] = build_nc()
    nc = _NC_CACHE["nc"]
    res = run_bass_kernel_spmd(nc, in_maps, core_ids=list(range(NCORES)))
    out = np.zeros((4, SEQ, D), np.float32)
    for c in range(NCORES):
        b, h = c // 2, c % 2
        out[b, h * OWN:(h + 1) * OWN] = res.results[c]["out"]
    kernel.last = res
    return out
```
